# Optimizing a Trainium2 kernel written in Bass

```python
import math
import jax, jax.numpy as jnp
from jax import lax
import numpy as np

D_MODEL = 4096
BATCH = 1
SEQ = 16384
DEPTH = 1
DEC_BATCH = 8
DEC_SEQ = 2048
PAST_LEN = 128

GRID_W = 64
N_HEADS = 16
N_KV_HEADS = 4
HEAD_DIM = 128
ATTN_WIDTH = N_HEADS * HEAD_DIM
KV_WIDTH = N_KV_HEADS * HEAD_DIM
Q_BLOCK = 128
ROPE_THETA = 10000.0
SSM_WIDTH = 2048
SSM_GROUP = 16
SSM_GROUPS = SSM_WIDTH // SSM_GROUP
SSM_STATE = 64
DT_MIN = 1e-3
DT_MAX = 1e-1
PEER_HEADS = 8
PEER_NKEYS = 128
PEER_EXPERTS = PEER_NKEYS * PEER_NKEYS
PEER_KEY_DIM = 256
PEER_HALF = PEER_KEY_DIM // 2
PEER_TOPK = 16
PEER_CHUNK = 128

EPS = 1e-6
IN_WIDTH = ATTN_WIDTH + 2 * KV_WIDTH + SSM_WIDTH + 2 * D_MODEL
SPLITS = [ATTN_WIDTH,
          ATTN_WIDTH + KV_WIDTH,
          ATTN_WIDTH + 2 * KV_WIDTH,
          ATTN_WIDTH + 2 * KV_WIDTH + SSM_WIDTH,
          ATTN_WIDTH + 2 * KV_WIDTH + SSM_WIDTH + D_MODEL]

kernel_name = "hybrid_s5_gqa_peer_encoder"


def rms_norm(x, g):
    xf = x.astype(jnp.float32)
    y = xf * lax.rsqrt(jnp.mean(xf * xf, axis=-1, keepdims=True) + EPS)
    return (y * g.astype(jnp.float32)).astype(x.dtype)


def axial_rope_tables(length):
    rows = length // GRID_W
    row = jnp.repeat(jnp.arange(rows), GRID_W).astype(jnp.float32)
    col = jnp.tile(jnp.arange(GRID_W), rows).astype(jnp.float32)
    axis_dim = HEAD_DIM // 2
    inv = ROPE_THETA ** (-jnp.arange(0, axis_dim, 2, dtype=jnp.float32) / axis_dim)
    ang = jnp.concatenate([row[:, None] * inv, col[:, None] * inv], axis=-1)
    return jnp.cos(ang), jnp.sin(ang)


def apply_axial_rope(x, cos, sin):
    b, l, h, d = x.shape
    q4 = d // 4
    xf = x.astype(jnp.float32).reshape(b, l, h, 2, 2, q4)
    x1 = xf[..., 0, :]
    x2 = xf[..., 1, :]
    c = cos.reshape(l, 1, 2, q4)
    s = sin.reshape(l, 1, 2, q4)
    o1 = x1 * c - x2 * s
    o2 = x1 * s + x2 * c
    return jnp.stack([o1, o2], axis=-2).reshape(b, l, h, d).astype(x.dtype)


def block_attention(q, k, v):
    b, l, _, d = q.shape
    n_blk = l // Q_BLOCK
    grp = N_HEADS // N_KV_HEADS
    scale = HEAD_DIM ** -0.5
    kf = k.astype(jnp.float32)
    vf = v.astype(jnp.float32)
    qb = q.reshape(b, n_blk, Q_BLOCK, N_KV_HEADS, grp, d).transpose(1, 0, 2, 3, 4, 5)

    def one_block(qblk):
        s = jnp.einsum('bqkgd,bskd->bkgqs', qblk.astype(jnp.float32), kf) * scale
        p = jax.nn.softmax(s, axis=-1)
        o = jnp.einsum('bkgqs,bskd->bqkgd', p, vf)
        return o.astype(q.dtype)

    o = lax.map(one_block, qb)
    return o.transpose(1, 0, 2, 3, 4, 5).reshape(b, l, N_HEADS * d)


def _linear_recurrence(e1, e2):
    a1, b1 = e1
    a2, b2 = e2
    return a1 * a2, a2 * b1 + b2


def ssm_scan(u_g, A_re, A_im, log_dt, B_re, B_im, C_re, C_im, reverse):
    l = u_g.shape[1]
    lam = lax.complex(A_re.astype(jnp.float32), A_im.astype(jnp.float32))
    dt = jnp.exp(log_dt.astype(jnp.float32))[:, None]
    lam_bar = jnp.exp(lam * dt)
    b_bar = lax.complex(B_re.astype(jnp.float32), B_im.astype(jnp.float32)) * ((lam_bar - 1.0) / lam)[..., None]
    bu = jnp.einsum('blhg,hpg->blhp', u_g.astype(jnp.complex64), b_bar)
    a = jnp.broadcast_to(lam_bar[None, None], (1, l) + lam_bar.shape)
    _, states = lax.associative_scan(_linear_recurrence, (a, bu), axis=1, reverse=reverse)
    c = lax.complex(C_re.astype(jnp.float32), C_im.astype(jnp.float32))
    return jnp.einsum('blhp,hgp->blhg', states, c).real


def ssm_branch(u, A_re, A_im, log_dt, B_re, B_im, C_re, C_im, D_skip, w_glu):
    b, l, _ = u.shape
    uf = u.astype(jnp.float32)
    ug = uf.reshape(b, l, SSM_GROUPS, SSM_GROUP)
    y = D_skip.astype(jnp.float32) * uf
    for dirn, rev in ((0, False), (1, True)):
        y = y + ssm_scan(ug, A_re[dirn], A_im[dirn], log_dt[dirn], B_re[dirn], B_im[dirn],
                         C_re[dirn], C_im[dirn], rev).reshape(b, l, SSM_WIDTH)
    z = jax.nn.gelu(y)
    out = z * jax.nn.sigmoid(z @ w_glu.astype(jnp.float32))
    return out.astype(u.dtype)


def peer(h, w_query, sub_keys, expert_u, expert_v):
    b, l, d = h.shape
    t = h.reshape(b * l, d)
    n = b * l
    q = (t @ w_query).astype(jnp.float32).reshape(n, PEER_HEADS, 2, PEER_HALF)
    s = jnp.einsum('nhcq,hckq->nhck', q, sub_keys.astype(jnp.float32))
    top_s, top_i = lax.top_k(s, PEER_TOPK)
    cand_s = (top_s[:, :, 0, :, None] + top_s[:, :, 1, None, :]).reshape(n, PEER_HEADS, PEER_TOPK * PEER_TOPK)
    cand_id = (top_i[:, :, 0, :, None] * PEER_NKEYS + top_i[:, :, 1, None, :]).reshape(n, PEER_HEADS, PEER_TOPK * PEER_TOPK)
    best_s, best_pos = lax.top_k(cand_s, PEER_TOPK)
    expert_id = jnp.take_along_axis(cand_id, best_pos, axis=-1)
    gate = jax.nn.softmax(best_s, axis=-1)
    n_chunk = n // PEER_CHUNK

    def one_chunk(args):
        tc, ids, g = args
        u = jnp.take(expert_u, ids, axis=0)
        v = jnp.take(expert_v, ids, axis=0)
        act = jax.nn.gelu(jnp.einsum('cd,chkd->chk', tc, u))
        return jnp.einsum('chk,chkd->cd', (g.astype(act.dtype) * act), v)

    y = lax.map(one_chunk, (t.reshape(n_chunk, PEER_CHUNK, d),
                            expert_id.reshape(n_chunk, PEER_CHUNK, PEER_HEADS, PEER_TOPK),
                            gate.reshape(n_chunk, PEER_CHUNK, PEER_HEADS, PEER_TOPK)))
    return y.reshape(b, l, d).astype(h.dtype)


def encoder_layer(x, norm_mix, w_in, q_norm, k_norm, ssm_A_re, ssm_A_im, ssm_log_dt,
                  ssm_B_re, ssm_B_im, ssm_C_re, ssm_C_im, ssm_D, w_glu, w_up_attn, w_up_ssm,
                  w_out, norm_ffn, peer_w_query, peer_sub_keys, peer_u, peer_v):
    b, l, _ = x.shape
    h = rms_norm(x, norm_mix)
    proj = h @ w_in
    q, k, v, u, g_attn, g_ssm = jnp.split(proj, SPLITS, axis=-1)
    q = rms_norm(q.reshape(b, l, N_HEADS, HEAD_DIM), q_norm)
    k = rms_norm(k.reshape(b, l, N_KV_HEADS, HEAD_DIM), k_norm)
    v = v.reshape(b, l, N_KV_HEADS, HEAD_DIM)
    cos, sin = axial_rope_tables(l)
    q = apply_axial_rope(q, cos, sin)
    k = apply_axial_rope(k, cos, sin)
    attn = block_attention(q, k, v)
    ssm = ssm_branch(u, ssm_A_re, ssm_A_im, ssm_log_dt, ssm_B_re, ssm_B_im,
                     ssm_C_re, ssm_C_im, ssm_D, w_glu)
    merged = jax.nn.sigmoid(g_attn) * (attn @ w_up_attn) + jax.nn.sigmoid(g_ssm) * (ssm @ w_up_ssm)
    x = x + merged @ w_out
    x = x + peer(rms_norm(x, norm_ffn), peer_w_query, peer_sub_keys, peer_u, peer_v)
    return x


def encoder_trunk(x, norm_mix, w_in, q_norm, k_norm, ssm_A_re, ssm_A_im, ssm_log_dt,
                  ssm_B_re, ssm_B_im, ssm_C_re, ssm_C_im, ssm_D, w_glu, w_up_attn, w_up_ssm,
                  w_out, norm_ffn, peer_w_query, peer_sub_keys, peer_u, peer_v, norm_final):
    for i in range(DEPTH):
        x = encoder_layer(x, norm_mix[i], w_in[i], q_norm[i], k_norm[i], ssm_A_re[i], ssm_A_im[i],
                          ssm_log_dt[i], ssm_B_re[i], ssm_B_im[i], ssm_C_re[i], ssm_C_im[i], ssm_D[i],
                          w_glu[i], w_up_attn[i], w_up_ssm[i], w_out[i], norm_ffn[i],
                          peer_w_query[i], peer_sub_keys[i], peer_u[i], peer_v[i])
    return rms_norm(x, norm_final)


def setup_inputs(seed: int = 0) -> dict:
    key = jax.random.key(seed)
    ks = jax.random.split(key, 26)
    f32 = jnp.float32

    def nrm(k, shape, scale):
        return jax.random.normal(k, shape, f32) * scale

    L = DEPTH
    H, P, G = SSM_GROUPS, SSM_STATE, SSM_GROUP
    n_idx = jnp.arange(P, dtype=f32)
    return {
        "x_prompt": nrm(ks[0], (BATCH, SEQ, D_MODEL), 1.0),
        "x_sample": nrm(ks[1], (DEC_BATCH, DEC_SEQ, D_MODEL), 1.0),
        "norm_mix": 1.0 + nrm(ks[2], (L, D_MODEL), 0.02),
        "w_in": nrm(ks[3], (L, D_MODEL, IN_WIDTH), D_MODEL ** -0.5),
        "q_norm": 1.0 + nrm(ks[4], (L, HEAD_DIM), 0.02),
        "k_norm": 1.0 + nrm(ks[5], (L, HEAD_DIM), 0.02),
        "ssm_A_re": -0.5 * jnp.exp(nrm(ks[6], (L, 2, H, P), 0.05)),
        "ssm_A_im": math.pi * n_idx + nrm(ks[7], (L, 2, H, P), 0.05),
        "ssm_log_dt": jax.random.uniform(ks[8], (L, 2, H), f32, math.log(DT_MIN), math.log(DT_MAX)),
        "ssm_B_re": nrm(ks[9], (L, 2, H, P, G), (2 * G) ** -0.5),
        "ssm_B_im": nrm(ks[10], (L, 2, H, P, G), (2 * G) ** -0.5),
        "ssm_C_re": nrm(ks[11], (L, 2, H, G, P), P ** -0.5),
        "ssm_C_im": nrm(ks[12], (L, 2, H, G, P), P ** -0.5),
        "ssm_D": nrm(ks[13], (L, SSM_WIDTH), 1.0),
        "w_glu": nrm(ks[14], (L, SSM_WIDTH, SSM_WIDTH), SSM_WIDTH ** -0.5),
        "w_up_attn": nrm(ks[15], (L, ATTN_WIDTH, D_MODEL), ATTN_WIDTH ** -0.5),
        "w_up_ssm": nrm(ks[16], (L, SSM_WIDTH, D_MODEL), SSM_WIDTH ** -0.5),
        "w_out": nrm(ks[17], (L, D_MODEL, D_MODEL), D_MODEL ** -0.5),
        "norm_ffn": 1.0 + nrm(ks[18], (L, D_MODEL), 0.02),
        "peer_w_query": nrm(ks[19], (L, D_MODEL, PEER_HEADS * PEER_KEY_DIM), D_MODEL ** -0.5),
        "peer_sub_keys": nrm(ks[20], (L, PEER_HEADS, 2, PEER_NKEYS, PEER_HALF), PEER_HALF ** -0.5),
        "peer_u": nrm(ks[21], (L, PEER_EXPERTS, D_MODEL), D_MODEL ** -0.5),
        "peer_v": nrm(ks[22], (L, PEER_EXPERTS, D_MODEL), PEER_HEADS ** -0.5),
        "norm_final": 1.0 + nrm(ks[23], (D_MODEL,), 0.02),
    }


def reference(x_prompt, x_sample, norm_mix, w_in, q_norm, k_norm, ssm_A_re, ssm_A_im, ssm_log_dt,
              ssm_B_re, ssm_B_im, ssm_C_re, ssm_C_im, ssm_D, w_glu, w_up_attn, w_up_ssm, w_out,
              norm_ffn, peer_w_query, peer_sub_keys, peer_u, peer_v, norm_final):
    y_prompt = encoder_trunk(x_prompt, norm_mix, w_in, q_norm, k_norm, ssm_A_re, ssm_A_im, ssm_log_dt,
                             ssm_B_re, ssm_B_im, ssm_C_re, ssm_C_im, ssm_D, w_glu, w_up_attn, w_up_ssm,
                             w_out, norm_ffn, peer_w_query, peer_sub_keys, peer_u, peer_v, norm_final)
    y_sample = encoder_trunk(x_sample, norm_mix, w_in, q_norm, k_norm, ssm_A_re, ssm_A_im, ssm_log_dt,
                             ssm_B_re, ssm_B_im, ssm_C_re, ssm_C_im, ssm_D, w_glu, w_up_attn, w_up_ssm,
                             w_out, norm_ffn, peer_w_query, peer_sub_keys, peer_u, peer_v, norm_final)
    return (y_prompt, y_sample)
```

```python
import numpy as np
import ml_dtypes
import concourse.bass as bass
import concourse.mybir as mybir
from concourse.bass_utils import run_bass_kernel_spmd

F32 = mybir.dt.float32
BF16 = mybir.dt.bfloat16
ALU = mybir.AluOpType
ACT = mybir.ActivationFunctionType
AX = mybir.AxisListType

EPS = 1e-6


class Prog:
    ENGS = ("sync", "scalar", "gpsimd", "tensor", "vector")

    def __init__(self, nc):
        self.nc = nc
        self.q = {e: [] for e in self.ENGS}
        self.cum = 0
        self.barrier = 0
        self.in_par = False

    def emit(self, eng, fn, dma=False):
        inc = 16 if dma else 1
        self.q[eng].append((fn, self.barrier, inc))
        self.cum += inc
        if not self.in_par:
            self.barrier = self.cum

    class _Par:
        def __init__(self, p):
            self.p = p

        def __enter__(self):
            self.p.in_par = True

        def __exit__(self, *a):
            self.p.in_par = False
            self.p.barrier = self.p.cum

    def par(self):
        return Prog._Par(self)

    def dma(self, out, in_, eng="sync", **kw):
        self.emit(eng, lambda e: e.dma_start(out=out, in_=in_, **kw), dma=True)

    def dma_cast(self, out, in_, **kw):
        self.emit("gpsimd", lambda e: e.dma_start(out=out, in_=in_, **kw), dma=True)

    def op(self, eng, name, *args, **kw):
        self.emit(eng, lambda e: getattr(e, name)(*args, **kw))

    def v(self, name, *args, **kw):
        self.op("vector", name, *args, **kw)

    def a(self, name, *args, **kw):
        self.op("scalar", name, *args, **kw)

    def g(self, name, *args, **kw):
        self.op("gpsimd", name, *args, **kw)

    def pe(self, name, *args, **kw):
        self.op("tensor", name, *args, **kw)

    def finish(self, G):
        nc = self.nc
        total = self.cum
        with nc.Block() as block:
            def run(engname):
                def body(e):
                    last = -1
                    for fn, w, inc in self.q[engname]:
                        if w > last and w > 0:
                            e.wait_ge(G, w)
                            last = w
                        fn(e).then_inc(G, inc)
                    e.wait_ge(G, total)
                return body
            block.sync(run("sync"))
            block.scalar(run("scalar"))
            block.gpsimd(run("gpsimd"))
            block.tensor(run("tensor"))
            block.vector(run("vector"))


def stage_norm_T(p, nc, ctx, x_src, g_src, hT_dst, N, D, ident):
    KC = D // 128
    TB = next(t for t in (512, 256, 128) if N % t == 0)
    gb = ctx.enter_context(nc.sbuf_tensor(nc.make_name("gb", True), [128, D], F32))
    xt = ctx.enter_context(nc.sbuf_tensor(nc.make_name("xt", True), [128, D], F32))
    xn = ctx.enter_context(nc.sbuf_tensor(nc.make_name("xn", True), [128, D], BF16))
    ss = ctx.enter_context(nc.sbuf_tensor(nc.make_name("ss", True), [128, 2], F32))
    hb = ctx.enter_context(nc.sbuf_tensor(nc.make_name("hb", True), [128, KC, TB], BF16))
    pt = ctx.enter_context(nc.psum_tensor(nc.make_name("ptn", True), [128, 8, 128], BF16))
    p.dma(gb[:], g_src.partition_broadcast(128))
    for b0 in range(0, N, TB):
        for ti in range(TB // 128):
            t0 = b0 + ti * 128
            p.dma(xt[:], x_src[t0:t0 + 128, :])
            p.a("activation", xn[:], xt[:], ACT.Square, accum_out=ss[:, 0:1])
            p.a("activation", ss[:, 1:2], ss[:, 0:1], ACT.Sqrt, bias=EPS, scale=1.0 / D)
            p.v("reciprocal", ss[:, 1:2], ss[:, 1:2])
            p.v("scalar_tensor_tensor", xn[:], xt[:], ss[:, 1:2], gb[:], ALU.mult, ALU.mult)
            for c0 in range(0, KC, 8):
                nb = min(8, KC - c0)
                with p.par():
                    for c in range(nb):
                        p.pe("transpose", pt[:, c, :], xn[:, (c0 + c) * 128:(c0 + c + 1) * 128], ident[:])
                p.v("tensor_copy", hb[:, c0:c0 + nb, ti * 128:(ti + 1) * 128], pt[:, 0:nb, :])
        p.dma(hT_dst[:, :, b0:b0 + TB].rearrange("k d n -> d k n"), hb[:])


def stage_gemm(p, nc, ctx, actT, w_src, N, K, M, orient, epi, tag, w_bf16=False):
    KC = K // 128
    TB = min(1024, N)
    ab = ctx.enter_context(nc.sbuf_tensor(nc.make_name("ab" + tag, True), [128, KC, TB], BF16))
    wt = ctx.enter_context(nc.sbuf_tensor(nc.make_name("wt" + tag, True), [128, KC, 512], BF16))
    ps = ctx.enter_context(nc.psum_tensor(nc.make_name("ps" + tag, True), [128, 512], F32))
    for tb in range(0, N, TB):
        with p.par():
            for k0 in range(0, KC, 8):
                k1 = min(KC, k0 + 8)
                p.dma(ab[:, k0:k1, :], actT[k0:k1, :, tb:tb + TB].rearrange("k d n -> d k n"))
        for mb in range(0, M, 512):
            mw = min(512, M - mb)
            with p.par():
                for k0 in range(0, KC, 8):
                    k1 = min(KC, k0 + 8)
                    wsrc_ap = w_src[k0 * 128:k1 * 128, mb:mb + mw].rearrange("(k d) m -> d k m", d=128)
                    if w_bf16:
                        p.dma(wt[:, k0:k1, 0:mw], wsrc_ap)
                    else:
                        p.dma_cast(wt[:, k0:k1, 0:mw], wsrc_ap)
            if orient == "T":
                for tt in range(0, TB, 128):
                    with p.par():
                        for kc in range(KC):
                            p.pe("matmul", ps[:, 0:mw], ab[:, kc, tt:tt + 128], wt[:, kc, 0:mw],
                                 start=(kc == 0), stop=(kc == KC - 1))
                    epi(ps[:, 0:mw], tb + tt, 128, mb, mw)
            else:
                for ms in range(0, mw, 128):
                    for ts in range(0, TB, 512):
                        tw = min(512, TB - ts)
                        with p.par():
                            for kc in range(KC):
                                p.pe("matmul", ps[:, 0:tw], wt[:, kc, ms:ms + 128], ab[:, kc, ts:ts + tw],
                                     start=(kc == 0), stop=(kc == KC - 1))
                        epi(ps[:, 0:tw], tb + ts, tw, mb + ms, 128)


def apv(t, off, dims):
    ps = 1
    for d in t.shape[1:]:
        ps *= d
    return bass.AP(t, off, [[ps, 128]] + [list(d) for d in dims])


class QKEpi:
    def __init__(self, p, nc, ctx, gvec_src, cos_src, sin_src, dstT, ident, tag):
        self.p, self.nc, self.dstT, self.ident = p, nc, dstT, ident
        self.cos_src, self.sin_src = cos_src, sin_src
        mk = lambda n, sh, dt: ctx.enter_context(nc.sbuf_tensor(nc.make_name(n + tag, True), sh, dt))
        self.t32 = mk("qe_t", [128, 512], F32)
        self.sq = mk("qe_s", [128, 512], F32)
        self.sq2 = mk("qe_s2", [128, 512], F32)
        self.ssh = mk("qe_h", [128, 8], F32)
        self.gqb = mk("qe_g", [128, 128], F32)
        self.cs = mk("qe_c", [128, 2, 64], F32)
        self.rb = mk("qe_r", [128, 512], BF16)
        self.qTs = mk("qe_q", [128, 4, 128], BF16)
        self.ptq = ctx.enter_context(nc.psum_tensor(nc.make_name("qe_p" + tag, True), [128, 4, 128], BF16))
        p.dma(self.gqb[:], gvec_src.partition_broadcast(128))
        self.cur_tok = None

    def __call__(self, ps, tok0, ntok, m0, nm):
        p = self.p
        nh = nm // 128
        t32, sq, sq2, ssh, rb = self.t32, self.sq, self.sq2, self.ssh, self.rb
        if self.cur_tok != tok0:
            with p.par():
                p.dma(self.cs[:, 0, :], self.cos_src[tok0:tok0 + 128, :])
                p.dma(self.cs[:, 1, :], self.sin_src[tok0:tok0 + 128, :])
            self.cur_tok = tok0
        p.v("tensor_copy", t32[:, 0:nm], ps)
        p.v("tensor_tensor", sq[:, 0:nm], t32[:, 0:nm], t32[:, 0:nm], ALU.mult)
        p.v("tensor_reduce", ssh[:, 0:nh], apv(sq, 0, [[128, nh], [1, 128]]), AX.X, ALU.add)
        p.a("activation", ssh[:, 4:4 + nh], ssh[:, 0:nh], ACT.Sqrt, bias=EPS, scale=1.0 / 128)
        p.v("reciprocal", ssh[:, 4:4 + nh], ssh[:, 4:4 + nh])
        v3 = lambda t: apv(t, 0, [[128, nh], [1, 128]])
        p.v("tensor_tensor", v3(t32), v3(t32), apv(ssh, 4, [[1, nh], [0, 128]]), ALU.mult)
        p.v("tensor_tensor", v3(t32), v3(t32), apv(self.gqb, 0, [[0, nh], [1, 128]]), ALU.mult)
        x = lambda t, half: apv(t, half * 32, [[128, nh], [64, 2], [1, 32]])
        cst = lambda which: apv(self.cs, which * 64, [[0, nh], [32, 2], [1, 32]])
        p.v("tensor_tensor", x(sq, 0), x(t32, 0), cst(0), ALU.mult)
        p.v("tensor_tensor", x(sq, 1), x(t32, 1), cst(1), ALU.mult)
        p.v("tensor_tensor", x(rb, 0), x(sq, 0), x(sq, 1), ALU.subtract)
        p.v("tensor_tensor", x(sq2, 0), x(t32, 0), cst(1), ALU.mult)
        p.v("tensor_tensor", x(sq2, 1), x(t32, 1), cst(0), ALU.mult)
        p.v("tensor_tensor", x(rb, 1), x(sq2, 0), x(sq2, 1), ALU.add)
        with p.par():
            for h in range(nh):
                p.pe("transpose", self.ptq[:, h, :], rb[:, h * 128:(h + 1) * 128], self.ident[:])
        p.v("tensor_copy", self.qTs[:, 0:nh, :], self.ptq[:, 0:nh, :])
        h0 = m0 // 128
        p.dma(self.dstT[h0:h0 + nh, :, tok0:tok0 + 128].rearrange("h d n -> d h n"), self.qTs[:, 0:nh, :])


def stage_attention(p, nc, ctx, qT_src, kT_src, v_src, attnT_dst, gq_src, gk_src, segs, NH, NKV, tag):
    GQ = NH // NKV
    NKmax = max(s[3] for s in segs)
    NQmax = max(s[1] for s in segs)
    mk = lambda n, sh, dt: ctx.enter_context(nc.sbuf_tensor(nc.make_name(n + tag, True), sh, dt))
    kT = mk("at_k", [128, NKmax], BF16)
    vv = mk("at_v", [128, NKmax // 128, 128], BF16)
    qTt = mk("at_q", [128, NQmax], BF16)
    pT = mk("at_p", [128, 4, 512], BF16)
    ones = mk("at_1", [128, 128], BF16)
    rc = mk("at_rc", [128, 512], F32)
    obb = mk("at_ob", [128, 512], BF16)
    gg = mk("at_g", [128, 2, 128], F32)
    cb = mk("at_cb", [128, 4], F32)
    sc = ctx.enter_context(nc.psum_tensor(nc.make_name("at_sc" + tag, True), [128, 4, 512], F32))
    acc_o = ctx.enter_context(nc.psum_tensor(nc.make_name("at_ao" + tag, True), [128, 512], F32))
    acc_s = ctx.enter_context(nc.psum_tensor(nc.make_name("at_as" + tag, True), [128, 512], F32))
    scale = 128 ** -0.5
    p.v("memset", ones[:], 1.0)
    with p.par():
        p.dma(gg[:, 0, :], gq_src.partition_broadcast(128))
        p.dma(gg[:, 1, :], gk_src.partition_broadcast(128))
    p.v("tensor_reduce", cb[:, 0:2], gg[:], AX.X, ALU.max, apply_absolute_value=True)
    p.v("tensor_tensor", cb[:, 2:3], cb[:, 0:1], cb[:, 1:2], ALU.mult)
    p.v("tensor_scalar", cb[:, 3:4], cb[:, 2:3], -(128 ** 0.5), None, ALU.mult)
    for (qoff, NQ, koff, NK) in segs:
        NKC = NK // 128
        for kvh in range(NKV):
            with p.par():
                p.dma(kT[:, 0:NK], kT_src[kvh, :, koff:koff + NK])
                p.dma(vv[:, 0:NKC, :], v_src[koff:koff + NK, kvh * 128:(kvh + 1) * 128].rearrange("(c p) d -> p c d", p=128))
            for qh in range(GQ):
                head = kvh * GQ + qh
                p.dma(qTt[:, 0:NQ], qT_src[head, :, qoff:qoff + NQ])
                for qb in range(0, NQ, 512):
                    qw = min(512, NQ - qb)
                    for kg in range(0, NKC, 4):
                        ng = min(4, NKC - kg)
                        with p.par():
                            for j in range(ng):
                                p.pe("matmul", sc[:, j, 0:qw], kT[:, (kg + j) * 128:(kg + j + 1) * 128],
                                     qTt[:, qb:qb + qw], start=True, stop=True)
                        p.a("activation", pT[:, 0:ng, 0:qw], sc[:, 0:ng, 0:qw], ACT.Exp, bias=cb[:, 3:4], scale=scale)
                        with p.par():
                            for j in range(ng):
                                first = (kg == 0 and j == 0)
                                last = (kg + j == NKC - 1)
                                p.pe("matmul", acc_o[:, 0:qw], vv[:, kg + j, :], pT[:, j, 0:qw], start=first, stop=last)
                                p.pe("matmul", acc_s[:, 0:qw], ones[:], pT[:, j, 0:qw], start=first, stop=last)
                    p.v("reciprocal", rc[:, 0:qw], acc_s[:, 0:qw])
                    p.v("tensor_tensor", obb[:, 0:qw], acc_o[:, 0:qw], rc[:, 0:qw], ALU.mult)
                    p.dma(attnT_dst[head, :, qoff + qb:qoff + qb + qw], obb[:, 0:qw])


import math


def ssm_host_layout(A_re, A_im, log_dt, B_re, B_im, C_re, C_im, D):
    _, H, P = A_re.shape
    Gs = B_re.shape[-1]
    NP = H // 2
    NJ = H * Gs // 128
    lay = lambda a: np.ascontiguousarray(a.reshape(2, NP, 2 * P).transpose(2, 0, 1))
    dte = np.ascontiguousarray(np.repeat(log_dt.reshape(2, NP, 2, 1), P, axis=3).reshape(2, NP, 2 * P).transpose(2, 0, 1))
    Bt = np.zeros((128, 2, 2, NJ, 128), np.float32)
    Bt3 = np.zeros((128, 2, 2, NJ, 128), np.float32)
    Ct = np.zeros((128, 2, 2, NP, 64), np.float32)
    for ri, (Bx, Cx) in enumerate(((B_re, C_re), (B_im, C_im))):
        for pair in range(NP):
            j, q = pair // 4, pair % 4
            for h2 in range(2):
                h = 2 * pair + h2
                blk = Bx[:, h].transpose(2, 0, 1)
                r0 = 32 * q + 16 * h2
                Bt[r0:r0 + Gs, :, ri, j, h2 * P:(h2 + 1) * P] = blk
                if q == 3:
                    Bt3[r0:r0 + Gs, :, ri, j, h2 * P:(h2 + 1) * P] = blk
                c0 = (q % 2) * 32 + 16 * h2
                Ct[h2 * P:(h2 + 1) * P, :, ri, pair, c0:c0 + Gs] = Cx[:, h].transpose(2, 0, 1)
    Dt = np.ascontiguousarray(D.reshape(NJ, 128).T)
    return dict(ssm_Ar=lay(A_re), ssm_Ai=lay(A_im), ssm_dt=dte, ssm_Bt=Bt, ssm_Bt3=Bt3, ssm_Ct=Ct, ssm_Dt=Dt)


def stage_ssm(p, nc, ctx, uT_hist, uT_own, zT_own, y_scr, prm, oh_src, LH, SEGL, seqs, NP, NJ, tag=""):
    from contextlib import ExitStack
    TBK = 64
    NSEG = LH // SEGL
    PPU = min(32, NP)
    mk = lambda n, sh, dt, c=ctx: c.enter_context(nc.sbuf_tensor(nc.make_name("ss_" + n + tag, True), sh, dt))
    A2 = mk("A2", [128, 2, 2, NP], F32); B2 = mk("B2", [128, 2, 2, NP], F32)
    Bt = mk("Bt", [128, 2, 2, NJ, 128], BF16); Bt3 = mk("Bt3", [128, 2, 2, NJ, 128], BF16)
    Cb = mk("Cb", [128, 2, 2, NP, 64], BF16)
    Dt = mk("Dt", [128, NJ], F32)
    oh = mk("oh", [128, 8], F32)
    Xc = mk("Xc", [128, 2, 2, NP], F32); Xin = mk("Xin", [128, 2, 2, NP], F32)
    m1 = mk("m1", [128, 2, 2, NP], F32); m2 = mk("m2", [128, 2, 2, NP], F32)
    with ExitStack() as c2:
        mk2 = lambda n, sh, dt: mk(n, sh, dt, c2)
        Ar = mk2("ar", [128, 2, NP], F32); Ai = mk2("ai", [128, 2, NP], F32); dtv = mk2("dt", [128, 2, NP], F32)
        t1 = mk2("t1", [128, 2, NP], F32); t2 = mk2("t2", [128, 2, NP], F32); t3 = mk2("t3", [128, 2, NP], F32)
        cc = mk2("cc", [128, 2, NP], F32); sn = mk2("sn", [128, 2, NP], F32)
        lbr = mk2("lbr", [128, 2, NP], F32); lbi = mk2("lbi", [128, 2, NP], F32)
        cr = mk2("cr", [128, 2, NP], F32); ci = mk2("ci", [128, 2, NP], F32)
        Ctf = mk2("Ctf", [128, 2, NP, 64], F32)
        W1 = mk2("W1", [128, NP, 64], F32); W2 = mk2("W2", [128, NP, 64], F32)
        with p.par():
            p.dma(Ar[:], prm["Ar"]); p.dma(Ai[:], prm["Ai"]); p.dma(dtv[:], prm["dt"])
            p.dma(Dt[:], prm["Dt"]); p.dma(oh[:], oh_src.partition_broadcast(128))
        for d in range(2):
            p.dma_cast(Bt[:, d], prm["Bt"][:, d])
            p.dma_cast(Bt3[:, d], prm["Bt3"][:, d])
        p.a("activation", dtv[:], dtv[:], ACT.Exp)
        p.v("tensor_tensor", t1[:], Ar[:], dtv[:], ALU.mult)
        p.v("tensor_tensor", t2[:], Ai[:], dtv[:], ALU.mult)
        p.a("activation", lbr[:], t1[:], ACT.Exp)
        p.a("activation", sn[:], t2[:], ACT.Sin, scale=1.0 / 32)
        p.v("tensor_scalar", t3[:], t2[:], 1.0 / 32, math.pi / 2, ALU.mult, ALU.add)
        p.a("activation", cc[:], t3[:], ACT.Sin)
        for _ in range(5):
            p.v("tensor_tensor", t1[:], cc[:], cc[:], ALU.mult)
            p.v("tensor_tensor", t2[:], sn[:], sn[:], ALU.mult)
            p.v("tensor_tensor", t3[:], cc[:], sn[:], ALU.mult)
            p.v("tensor_tensor", cc[:], t1[:], t2[:], ALU.subtract)
            p.v("tensor_scalar", sn[:], t3[:], 2.0, None, ALU.mult)
        p.v("tensor_tensor", lbi[:], lbr[:], sn[:], ALU.mult)
        p.v("tensor_tensor", lbr[:], lbr[:], cc[:], ALU.mult)
        for ri in range(2):
            p.v("tensor_copy", A2[:, :, ri, :], lbr[:])
        p.v("tensor_scalar", B2[:, :, 0, :], lbi[:], -1.0, None, ALU.mult)
        p.v("tensor_copy", B2[:, :, 1, :], lbi[:])
        p.v("tensor_scalar", t1[:], lbr[:], -1.0, None, ALU.add)
        p.v("tensor_tensor", t2[:], Ar[:], Ar[:], ALU.mult)
        p.v("tensor_tensor", t3[:], Ai[:], Ai[:], ALU.mult)
        p.v("tensor_tensor", t2[:], t2[:], t3[:], ALU.add)
        p.v("reciprocal", t2[:], t2[:])
        p.v("tensor_tensor", cr[:], t1[:], Ar[:], ALU.mult)
        p.v("tensor_tensor", t3[:], lbi[:], Ai[:], ALU.mult)
        p.v("tensor_tensor", cr[:], cr[:], t3[:], ALU.add)
        p.v("tensor_tensor", cr[:], cr[:], t2[:], ALU.mult)
        p.v("tensor_tensor", ci[:], lbi[:], Ar[:], ALU.mult)
        p.v("tensor_tensor", t3[:], t1[:], Ai[:], ALU.mult)
        p.v("tensor_tensor", ci[:], ci[:], t3[:], ALU.subtract)
        p.v("tensor_tensor", ci[:], ci[:], t2[:], ALU.mult)
        for d in range(2):
            p.dma(Ctf[:], prm["Ct"][:, d])
            bc = lambda t: apv(t, d * NP, [[1, NP], [0, 64]])
            p.v("tensor_tensor", W1[:], Ctf[:, 0], bc(cr), ALU.mult)
            p.v("tensor_tensor", W2[:], Ctf[:, 1], bc(ci), ALU.mult)
            p.v("tensor_tensor", Cb[:, d, 0], W1[:], W2[:], ALU.subtract)
            p.v("tensor_tensor", W1[:], Ctf[:, 0], bc(ci), ALU.mult)
            p.v("tensor_tensor", W2[:], Ctf[:, 1], bc(cr), ALU.mult)
            p.v("tensor_tensor", W1[:], W1[:], W2[:], ALU.add)
            p.v("tensor_scalar", Cb[:, d, 1], W1[:], -1.0, None, ALU.mult)
    XB = mk("XB", [128, 2, 2, NP, TBK], F32)
    Xbf = mk("Xbf", [128, 2, NP, TBK], BF16)
    uf = mk("uf", [128, NJ, TBK], BF16); ub = mk("ub", [128, NJ, TBK], BF16)
    yt = mk("yt", [128, NJ, TBK], F32); yp = mk("yp", [128, NJ, TBK], F32); zb = mk("zb", [128, NJ, TBK], BF16)
    psu = ctx.enter_context(nc.psum_tensor(nc.make_name("ss_psu" + tag, True), [128, PPU, TBK], F32))
    psy = ctx.enter_context(nc.psum_tensor(nc.make_name("ss_psy" + tag, True), [128, NJ, TBK], F32))
    RS = NP * TBK
    DS = 2 * RS
    PS = 2 * DS

    def slot(sf, sb):
        return bass.AP(XB, sf, [[PS, 128], [DS + sb - sf, 2], [RS, 2], [TBK, NP]])

    def slot_ri(sf, sb, ri):
        return bass.AP(XB, sf + ri * RS, [[PS, 128], [DS + sb - sf, 2], [TBK, NP]])

    def block_step(usrc, tf0, tb0):
        with p.par():
            p.dma(uf[:], usrc[:, :, tf0:tf0 + TBK].rearrange("j c t -> c j t"))
            p.dma(ub[:], usrc[:, :, tb0:tb0 + TBK].rearrange("j c t -> c j t"))
        for d in range(2):
            u = uf if d == 0 else ub
            for ri in range(2):
                for pb in range(0, NP, PPU):
                    jb, nj = pb // 4, PPU // 4
                    for q in range(4):
                        with p.par():
                            for jj in range(nj):
                                j = jb + jj
                                if q < 3:
                                    lhsT = Bt[32 * q:32 * q + 32, d, ri, j, :]; rhs = u[32 * q:32 * q + 32, j, :]
                                else:
                                    lhsT = Bt3[64:128, d, ri, j, :]; rhs = u[64:128, j, :]
                                p.pe("matmul", psu[:, 4 * jj + q, :], lhsT, rhs, start=True, stop=True)
                    p.a("copy", XB[:, d, ri, pb:pb + PPU, :], psu[:, 0:PPU, :])
        with p.par():
            for s_ in range(TBK):
                prev = Xc[:] if s_ == 0 else slot(s_ - 1, TBK - s_)
                pr = (lambda r: Xc[:, :, r, :]) if s_ == 0 else (lambda r: slot_ri(s_ - 1, TBK - s_, r))
                cur = slot(s_, TBK - 1 - s_)
                p.v("tensor_tensor", m1[:], prev, A2[:], ALU.mult)
                p.v("tensor_tensor", m2[:, :, 0, :], pr(1), B2[:, :, 0, :], ALU.mult)
                p.v("tensor_tensor", m2[:, :, 1, :], pr(0), B2[:, :, 1, :], ALU.mult)
                p.v("tensor_tensor", m1[:], m1[:], m2[:], ALU.add)
                p.v("tensor_tensor", cur, m1[:], cur, ALU.add)
        p.v("tensor_copy", Xc[:], slot(TBK - 1, 0))

    p.v("memset", Xin[:], 0.0)
    p.v("memset", Xc[:], 0.0)
    if NSEG > 1:
        for n in range(LH // TBK):
            if (TBK * n) >= LH - SEGL:
                break
            block_step(uT_hist, TBK * n, LH - TBK * (n + 1))
            done = TBK * (n + 1)
            if done % SEGL == 0:
                k = done // SEGL
                p.v("scalar_tensor_tensor", Xin[:, 0], Xc[:, 0], oh[:, k:k + 1], Xin[:, 0], ALU.mult, ALU.add)
                kb = NSEG - 1 - k
                p.v("scalar_tensor_tensor", Xin[:, 1], Xc[:, 1], oh[:, kb:kb + 1], Xin[:, 1], ALU.mult, ALU.add)
    NB = SEGL // TBK
    for (o, use_hist) in seqs:
        if use_hist:
            p.v("tensor_copy", Xc[:], Xin[:])
        else:
            p.v("memset", Xc[:], 0.0)
        for n in range(NB):
            tf0 = o + TBK * n
            tb0 = o + SEGL - TBK * (n + 1)
            block_step(uT_own, tf0, tb0)
            for d in range(2):
                tok0 = tf0 if d == 0 else tb0
                u = uf if d == 0 else ub
                p.a("copy", Xbf[:], XB[:, d])
                for half in range(2):
                    with p.par():
                        for j in range(NJ):
                            for qq in range(2):
                                pair = 4 * j + 2 * half + qq
                                for ri in range(2):
                                    p.pe("matmul", psy[64 * half:64 * half + 64, j, :], Cb[:, d, ri, pair, :],
                                         Xbf[:, ri, pair, :], start=(qq == 0 and ri == 0), stop=(qq == 1 and ri == 1))
                dst = y_scr[:, :, tok0:tok0 + TBK].rearrange("j c t -> c j t")
                if n < NB // 2:
                    p.v("tensor_copy", yt[:], psy[:])
                    p.dma(dst, yt[:])
                else:
                    p.dma(yp[:], dst)
                    p.v("tensor_tensor", yt[:], psy[:], yp[:], ALU.add)
                    p.v("tensor_tensor", yp[:], u[:], apv(Dt, 0, [[1, NJ], [0, TBK]]), ALU.mult)
                    p.v("tensor_tensor", yt[:], yt[:], yp[:], ALU.add)
                    p.a("activation", zb[:], yt[:], ACT.Gelu_apprx_tanh)
                    p.dma(zT_own[:, :, tok0:tok0 + TBK].rearrange("j c t -> c j t"), zb[:])


def stage_gemm_bigk(p, nc, ctx, actT, w_src, N, K, M, epi, tag):
    KC = K // 128
    KP = min(16, KC)
    TB = min(1024, N)
    NT = TB // 128
    ab = ctx.enter_context(nc.sbuf_tensor(nc.make_name("bk_a" + tag, True), [128, KP, TB], BF16))
    wt = ctx.enter_context(nc.sbuf_tensor(nc.make_name("bk_w" + tag, True), [128, KP, 512], BF16))
    ps = ctx.enter_context(nc.psum_tensor(nc.make_name("bk_p" + tag, True), [128, NT, 512], F32))
    for tb in range(0, N, TB):
        for mb in range(0, M, 512):
            mw = min(512, M - mb)
            for k0 in range(0, KC, KP):
                with p.par():
                    for kk in range(0, KP, 8):
                        p.dma(ab[:, kk:kk + 8, :], actT[k0 + kk:k0 + kk + 8, :, tb:tb + TB].rearrange("k d n -> d k n"))
                with p.par():
                    for kk in range(0, KP, 8):
                        p.dma_cast(wt[:, kk:kk + 8, 0:mw],
                                   w_src[(k0 + kk) * 128:(k0 + kk + 8) * 128, mb:mb + mw].rearrange("(k d) m -> d k m", d=128))
                with p.par():
                    for t in range(NT):
                        for kc in range(KP):
                            p.pe("matmul", ps[:, t, 0:mw], ab[:, kc, t * 128:(t + 1) * 128], wt[:, kc, 0:mw],
                                 start=(k0 == 0 and kc == 0), stop=(k0 + kc == KC - 1))
            for t in range(NT):
                epi(ps[:, t, 0:mw], tb + t * 128, 128, mb, mw)


def stage_peer_ut(p, nc, ctx, U_src, UT_d, NE, D, ident):
    KC = D // 128
    ur = ctx.enter_context(nc.sbuf_tensor("pu_r", [128, D], BF16))
    us = ctx.enter_context(nc.sbuf_tensor("pu_s", [128, KC, 512], BF16))
    pt = ctx.enter_context(nc.psum_tensor("pu_p", [128, 8, 128], BF16))
    for e0 in range(0, NE, 512):
        for eb in range(4):
            p.dma_cast(ur[:], U_src[e0 + eb * 128:e0 + (eb + 1) * 128, :])
            for c0 in range(0, KC, 8):
                nb = min(8, KC - c0)
                with p.par():
                    for c in range(nb):
                        p.pe("transpose", pt[:, c, :], ur[:, (c0 + c) * 128:(c0 + c + 1) * 128], ident[:])
                p.v("tensor_copy", us[:, c0:c0 + nb, eb * 128:(eb + 1) * 128], pt[:, 0:nb, :])
        p.dma(UT_d[:, e0:e0 + 512].rearrange("(k d) e -> d k e", d=128), us[:])


def stage_peer_gate(p, nc, ctx, qT_d, skT_src, G_d, N, PH, tag=""):
    HC = 2 * PH
    mk = lambda n, sh, dt: ctx.enter_context(nc.sbuf_tensor(nc.make_name("pg_" + n + tag, True), sh, dt))
    skT = mk("sk", [128, HC, 128], BF16)
    qt = mk("q", [128, HC, 128], BF16)
    S = mk("S", [128, HC, 128], F32)
    wk = mk("wk", [128, 256], F32)
    top1 = mk("t1", [128, HC, 16], F32)
    cand = mk("cd", [128, PH, 256], F32)
    top2 = mk("t2", [128, PH, 16], F32)
    st = mk("st", [128, 4, PH], F32)
    ex = mk("ex", [128, PH, 16], F32)
    T1 = mk("T1", [128, 16, 128], F32)
    Mk = mk("Mk", [128, 16, 128], F32)
    Gf = mk("Gf", [128, 16, 128], F32)
    Gb = mk("Gb", [128, 128 * 128], BF16)
    ps = ctx.enter_context(nc.psum_tensor(nc.make_name("pg_ps" + tag, True), [128, HC, 128], F32))
    p.dma_cast(skT[:], skT_src)
    for t0 in range(0, N, 128):
        p.dma(qt[:], qT_d[:, :, t0:t0 + 128].rearrange("c q n -> q c n"))
        with p.par():
            for hc in range(HC):
                p.pe("matmul", ps[:, hc, :], qt[:, hc, :], skT[:, hc, :], start=True, stop=True)
        p.v("tensor_copy", S[:], ps[:])
        for hc in range(HC):
            p.v("max", out=top1[:, hc, 0:8], in_=S[:, hc, :])
            p.v("match_replace", out=wk[:, 0:128], in_to_replace=top1[:, hc, 0:8], in_values=S[:, hc, :], imm_value=-1e30)
            p.v("max", out=top1[:, hc, 8:16], in_=wk[:, 0:128])
        p.v("tensor_tensor", apv(cand, 0, [[256, PH], [16, 16], [1, 16]]), apv(top1, 0, [[32, PH], [1, 16], [0, 16]]),
            apv(top1, 16, [[32, PH], [0, 16], [1, 16]]), ALU.add)
        for h in range(PH):
            p.v("max", out=top2[:, h, 0:8], in_=cand[:, h, :])
            p.v("match_replace", out=wk[:], in_to_replace=top2[:, h, 0:8], in_values=cand[:, h, :], imm_value=-1e30)
            p.v("max", out=top2[:, h, 8:16], in_=wk[:])
        p.v("tensor_copy", st[:, 0, :], top2[:, :, 15])
        p.v("tensor_tensor", ex[:], top2[:], apv(top2, 0, [[16, PH], [0, 16]]), ALU.subtract)
        p.a("activation", ex[:], ex[:], ACT.Exp)
        p.v("tensor_reduce", st[:, 2, :], ex[:], AX.X, ALU.add)
        p.a("activation", st[:, 3, :], st[:, 2, :], ACT.Ln)
        p.v("tensor_tensor", st[:, 1, :], st[:, 3, :], top2[:, :, 0], ALU.add)
        p.v("tensor_scalar", st[:, 1, :], st[:, 1, :], -1.0, None, ALU.mult)
        for g0 in range(0, 128, 16):
            for h in range(PH):
                s2b = apv(S, (2 * h + 1) * 128, [[0, 16], [1, 128]])
                s1g = apv(S, 2 * h * 128 + g0, [[1, 16], [0, 128]])
                p.v("tensor_tensor", Mk[:], s1g, s2b, ALU.add)
                p.a("activation", T1[:], Mk[:], ACT.Exp, bias=st[:, 1, h:h + 1])
                if h == 0:
                    p.v("scalar_tensor_tensor", Gf[:], Mk[:], st[:, 0, h:h + 1], T1[:], ALU.is_ge, ALU.mult)
                else:
                    p.v("scalar_tensor_tensor", T1[:], Mk[:], st[:, 0, h:h + 1], T1[:], ALU.is_ge, ALU.mult)
                    p.v("tensor_tensor", Gf[:], Gf[:], T1[:], ALU.add)
            p.a("copy", Gb[:, g0 * 128:(g0 + 16) * 128], Gf[:].rearrange("p a b -> p (a b)"))
        with p.par():
            for c in range(0, 128 * 128, 4096):
                p.dma(G_d[t0:t0 + 128, c:c + 4096], Gb[:, c:c + 4096])


def stage_norm_tok(p, nc, ctx, x_src, g_src, y_dst, N, D):
    gb = ctx.enter_context(nc.sbuf_tensor(nc.make_name("fgb", True), [128, D], F32))
    xt = ctx.enter_context(nc.sbuf_tensor(nc.make_name("fxt", True), [128, D], F32))
    xn = ctx.enter_context(nc.sbuf_tensor(nc.make_name("fxn", True), [128, D], F32))
    ss = ctx.enter_context(nc.sbuf_tensor(nc.make_name("fss", True), [128, 2], F32))
    p.dma(gb[:], g_src.partition_broadcast(128))
    for t0 in range(0, N, 128):
        p.dma(xt[:], x_src[t0:t0 + 128, :])
        p.a("activation", xn[:], xt[:], ACT.Square, accum_out=ss[:, 0:1])
        p.a("activation", ss[:, 1:2], ss[:, 0:1], ACT.Sqrt, bias=EPS, scale=1.0 / D)
        p.v("reciprocal", ss[:, 1:2], ss[:, 1:2])
        p.v("scalar_tensor_tensor", xn[:], xt[:], ss[:, 1:2], gb[:], ALU.mult, ALU.mult)
        p.dma(y_dst[t0:t0 + 128, :], xn[:])


def build_program(cfg):
    from contextlib import ExitStack
    D = cfg["D"]; NOWN = cfg["NOWN"]; SEGL = cfg["SEGL"]; LH = cfg["LH"]; NKV = LH + SEGL
    NH = cfg["NH"]; NKVH = cfg["NKVH"]; SW = cfg["SW"]; PH = cfg["PH"]; NE = cfg["NE"]
    NP = SW // 32; NJ = SW // 128
    AW = NH * 128; KW = NKVH * 128
    INW = AW + 2 * KW + SW + 2 * D
    cU = AW + 2 * KW; cG = cU + SW
    DC = D // 128
    nc = bass.Bass("TRN2", target_bir_lowering=False)
    din = lambda n, sh: nc.dram_tensor(n, sh, F32, kind="ExternalInput").ap()
    x_own = din("x_own", [NOWN, D]); x_kv = din("x_kv", [NKV, D])
    norm_mix = din("norm_mix", [D]); norm_ffn = din("norm_ffn", [D]); norm_final = din("norm_final", [D])
    w_in = din("w_in", [D, INW]); gq = din("q_norm", [128]); gk = din("k_norm", [128])
    cos_q = din("cos_q", [NOWN, 64]); sin_q = din("sin_q", [NOWN, 64])
    cos_k = din("cos_k", [NKV, 64]); sin_k = din("sin_k", [NKV, 64])
    ident_in = din("ident", [128, 128]); oh_src = din("onehot", [8])
    prm = dict(Ar=din("ssm_Ar", [128, 2, NP]), Ai=din("ssm_Ai", [128, 2, NP]), dt=din("ssm_dt", [128, 2, NP]),
               Bt=din("ssm_Bt", [128, 2, 2, NJ, 128]), Bt3=din("ssm_Bt3", [128, 2, 2, NJ, 128]),
               Ct=din("ssm_Ct", [128, 2, 2, NP, 64]), Dt=din("ssm_Dt", [128, NJ]))
    w_glu = din("w_glu", [SW, SW]); w_up_attn = din("w_up_attn", [AW, D]); w_up_ssm = din("w_up_ssm", [SW, D])
    w_out = din("w_out", [D, D]); w_query = din("peer_w_query", [D, PH * 256])
    skT_src = din("peer_skT", [128, 2 * PH, 128]); U_src = din("peer_u", [NE, D]); V_src = din("peer_v", [NE, D])
    dbg = cfg.get("debug", False)
    y_out = nc.dram_tensor("y_out", [NOWN, D], F32, kind="ExternalOutput").ap()

    def scr(n, sh, dt=BF16):
        return nc.dram_tensor(n, sh, dt, kind=("ExternalOutput" if dbg else "Internal")).ap()
    hT_own = scr("hT_own", [DC, 128, NOWN]); hT_kv = scr("hT_kv", [DC, 128, NKV])
    qT = scr("qT", [NH, 128, NOWN]); kT = scr("kT", [NKVH, 128, NKV]); vt = scr("vt", [NKV, KW])
    attnT = scr("attnT", [NH, 128, NOWN])
    uT_hist = scr("uT_hist", [NJ, 128, LH]); uT_own = scr("uT_own", [NJ, 128, NOWN])
    zT = scr("zT", [NJ, 128, NOWN]); y_scr = scr("y_scr", [NJ, 128, NOWN], F32)
    soT = scr("soT", [NJ, 128, NOWN]); sgT = scr("sgT", [2 * DC, 128, NOWN])
    mF = scr("mF", [DC, 128, NOWN], F32); mergedT = scr("mergedT", [DC, 128, NOWN])
    x2 = scr("x2", [NOWN, D], F32); tT = scr("tT", [DC, 128, NOWN]); qpT = scr("qpT", [2 * PH, 128, NOWN])
    G_d = scr("G_d", [NOWN, NE]); UT_d = scr("UT_d", [D, NE]); AT_d = scr("AT_d", [NE // 128, 128, NOWN])
    x3 = scr("x3", [NOWN, D], F32)
    with ExitStack() as ctx:
        G = ctx.enter_context(nc.semaphore("G"))
        ident = ctx.enter_context(nc.sbuf_tensor("identb", [128, 128], BF16))
        p = Prog(nc)
        p.dma_cast(ident[:], ident_in)

        def store_T_epi(sctx, dst, dt, name, func=None):
            stt = sctx.enter_context(nc.sbuf_tensor(name, [128, 512], dt))

            def epi(ps, tok0, ntok, m0, nm):
                if func is None:
                    p.v("tensor_copy", stt[:, 0:ntok], ps)
                else:
                    p.a("activation", stt[:, 0:ntok], ps, func)
                p.dma(dst[m0 // 128, :, tok0:tok0 + ntok], stt[:, 0:ntok])
            return epi

        with ExitStack() as sctx:
            stage_norm_T(p, nc, sctx, x_own, norm_mix, hT_own, NOWN, D, ident)
        with ExitStack() as sctx:
            stage_norm_T(p, nc, sctx, x_kv, norm_mix, hT_kv, NKV, D, ident)
        with ExitStack() as sctx:
            epq = QKEpi(p, nc, sctx, gq, cos_q, sin_q, qT, ident, "q")
            stage_gemm(p, nc, sctx, hT_own, w_in[:, 0:AW], NOWN, D, AW, "T", epq, "q")
        with ExitStack() as sctx:
            epk = QKEpi(p, nc, sctx, gk, cos_k, sin_k, kT, ident, "k")
            stage_gemm(p, nc, sctx, hT_kv, w_in[:, AW:AW + KW], NKV, D, KW, "T", epk, "k")
        with ExitStack() as sctx:
            st = sctx.enter_context(nc.sbuf_tensor("stv", [128, 512], BF16))

            def epv(ps, tok0, ntok, m0, nm):
                p.v("tensor_copy", st[:, 0:nm], ps)
                p.dma(vt[tok0:tok0 + ntok, m0:m0 + nm], st[:, 0:nm])
            stage_gemm(p, nc, sctx, hT_kv, w_in[:, AW + KW:AW + 2 * KW], NKV, D, KW, "T", epv, "v")
        with ExitStack() as sctx:
            stage_attention(p, nc, sctx, qT, kT, vt, attnT, gq, gk, cfg["segs"], NH, NKVH, "a")
        if LH > SEGL:
            with ExitStack() as sctx:
                stage_gemm(p, nc, sctx, hT_kv[:, :, 0:LH], w_in[:, cU:cU + SW], LH, D, SW, "F",
                           store_T_epi(sctx, uT_hist, BF16, "st_uh"), "uh")
        with ExitStack() as sctx:
            stage_gemm(p, nc, sctx, hT_own, w_in[:, cU:cU + SW], NOWN, D, SW, "F",
                       store_T_epi(sctx, uT_own, BF16, "st_uo"), "uo")
        with ExitStack() as sctx:
            stage_ssm(p, nc, sctx, uT_hist, uT_own, zT, y_scr, prm, oh_src, LH, SEGL,
                      [(0, True), (SEGL, False)], NP, NJ)
        with ExitStack() as sctx:
            sg = sctx.enter_context(nc.sbuf_tensor("glu_s", [128, 512], F32))
            zt_ = sctx.enter_context(nc.sbuf_tensor("glu_z", [128, 512], BF16))
            so_ = sctx.enter_context(nc.sbuf_tensor("glu_o", [128, 512], BF16))

            def epi_glu(ps, tok0, ntok, m0, nm):
                p.a("activation", sg[:, 0:ntok], ps, ACT.Sigmoid)
                p.dma(zt_[:, 0:ntok], zT[m0 // 128, :, tok0:tok0 + ntok])
                p.v("tensor_tensor", so_[:, 0:ntok], zt_[:, 0:ntok], sg[:, 0:ntok], ALU.mult)
                p.dma(soT[m0 // 128, :, tok0:tok0 + ntok], so_[:, 0:ntok])
            stage_gemm(p, nc, sctx, zT, w_glu, NOWN, SW, SW, "F", epi_glu, "glu")
        with ExitStack() as sctx:
            stage_gemm(p, nc, sctx, hT_own, w_in[:, cG:cG + 2 * D], NOWN, D, 2 * D, "F",
                       store_T_epi(sctx, sgT, BF16, "st_sg", ACT.Sigmoid), "sg")
        with ExitStack() as sctx:
            ga = sctx.enter_context(nc.sbuf_tensor("ua_g", [128, 512], BF16))
            mo = sctx.enter_context(nc.sbuf_tensor("ua_m", [128, 512], F32))

            def epi_ua(ps, tok0, ntok, m0, nm):
                p.dma(ga[:, 0:ntok], sgT[m0 // 128, :, tok0:tok0 + ntok])
                p.v("tensor_tensor", mo[:, 0:ntok], ps, ga[:, 0:ntok], ALU.mult)
                p.dma(mF[m0 // 128, :, tok0:tok0 + ntok], mo[:, 0:ntok])
            stage_gemm(p, nc, sctx, attnT, w_up_attn, NOWN, AW, D, "F", epi_ua, "ua")
        with ExitStack() as sctx:
            gs = sctx.enter_context(nc.sbuf_tensor("us_g", [128, 512], BF16))
            mp = sctx.enter_context(nc.sbuf_tensor("us_p", [128, 512], F32))
            mo2 = sctx.enter_context(nc.sbuf_tensor("us_m", [128, 512], F32))
            mb_ = sctx.enter_context(nc.sbuf_tensor("us_b", [128, 512], BF16))

            def epi_us(ps, tok0, ntok, m0, nm):
                with p.par():
                    p.dma(gs[:, 0:ntok], sgT[DC + m0 // 128, :, tok0:tok0 + ntok])
                    p.dma(mp[:, 0:ntok], mF[m0 // 128, :, tok0:tok0 + ntok])
                p.v("tensor_tensor", mo2[:, 0:ntok], ps, gs[:, 0:ntok], ALU.mult)
                p.v("tensor_tensor", mb_[:, 0:ntok], mo2[:, 0:ntok], mp[:, 0:ntok], ALU.add)
                p.dma(mergedT[m0 // 128, :, tok0:tok0 + ntok], mb_[:, 0:ntok])
            stage_gemm(p, nc, sctx, soT, w_up_ssm, NOWN, SW, D, "F", epi_us, "us")
        with ExitStack() as sctx:
            xr = sctx.enter_context(nc.sbuf_tensor("wo_x", [128, 512], F32))
            xo = sctx.enter_context(nc.sbuf_tensor("wo_o", [128, 512], F32))

            def epi_wo(ps, tok0, ntok, m0, nm):
                p.dma(xr[:, 0:nm], x_own[tok0:tok0 + ntok, m0:m0 + nm])
                p.v("tensor_tensor", xo[:, 0:nm], ps, xr[:, 0:nm], ALU.add)
                p.dma(x2[tok0:tok0 + ntok, m0:m0 + nm], xo[:, 0:nm])
            stage_gemm(p, nc, sctx, mergedT, w_out, NOWN, D, D, "T", epi_wo, "wo")
        with ExitStack() as sctx:
            stage_norm_T(p, nc, sctx, x2, norm_ffn, tT, NOWN, D, ident)
        with ExitStack() as sctx:
            stage_peer_ut(p, nc, sctx, U_src, UT_d, NE, D, ident)
        with ExitStack() as sctx:
            stage_gemm(p, nc, sctx, tT, w_query, NOWN, D, PH * 256, "F",
                       store_T_epi(sctx, qpT, BF16, "st_qp"), "qp")
        with ExitStack() as sctx:
            stage_peer_gate(p, nc, sctx, qpT, skT_src, G_d, NOWN, PH)
        with ExitStack() as sctx:
            ge = sctx.enter_context(nc.sbuf_tensor("pa_ge", [128, 512], F32))
            gl = sctx.enter_context(nc.sbuf_tensor("pa_gl", [128, 512], BF16))
            ab_ = sctx.enter_context(nc.sbuf_tensor("pa_a", [128, 512], BF16))
            at_ = sctx.enter_context(nc.sbuf_tensor("pa_at", [128, 4, 128], BF16))
            pta = sctx.enter_context(nc.psum_tensor("pa_pt", [128, 4, 128], BF16))

            def epi_pa(ps, tok0, ntok, m0, nm):
                p.a("activation", ge[:, 0:nm], ps, ACT.Gelu_apprx_tanh)
                p.dma(gl[:, 0:nm], G_d[tok0:tok0 + ntok, m0:m0 + nm])
                p.v("tensor_tensor", ab_[:, 0:nm], ge[:, 0:nm], gl[:, 0:nm], ALU.mult)
                nb = nm // 128
                with p.par():
                    for c in range(nb):
                        p.pe("transpose", pta[:, c, :], ab_[:, c * 128:(c + 1) * 128], ident[:])
                p.v("tensor_copy", at_[:, 0:nb, :], pta[:, 0:nb, :])
                p.dma(AT_d[m0 // 128:m0 // 128 + nb, :, tok0:tok0 + ntok].rearrange("c e t -> e c t"), at_[:, 0:nb, :])
            stage_gemm(p, nc, sctx, tT, UT_d, NOWN, D, NE, "T", epi_pa, "pa", w_bf16=True)
        with ExitStack() as sctx:
            xr2 = sctx.enter_context(nc.sbuf_tensor("pb_x", [128, 512], F32))
            xo2 = sctx.enter_context(nc.sbuf_tensor("pb_o", [128, 512], F32))

            def epi_pb(ps, tok0, ntok, m0, nm):
                p.dma(xr2[:, 0:nm], x2[tok0:tok0 + ntok, m0:m0 + nm])
                p.v("tensor_tensor", xo2[:, 0:nm], ps, xr2[:, 0:nm], ALU.add)
                p.dma(x3[tok0:tok0 + ntok, m0:m0 + nm], xo2[:, 0:nm])
            stage_gemm_bigk(p, nc, sctx, AT_d, V_src, NOWN, NE, D, epi_pb, "pb")
        with ExitStack() as sctx:
            stage_norm_tok(p, nc, sctx, x3, norm_final, y_out, NOWN, D)
        p.finish(G)
    return nc


FULL = dict(D=4096, NOWN=4096, SEGL=2048, LH=16384, NH=16, NKVH=4, SW=2048, PH=8, NE=16384,
            segs=[(0, 2048, 0, 16384), (2048, 2048, 16384, 2048)])


def _rope_tables(length):
    rows = length // 64
    row = np.repeat(np.arange(rows), 64).astype(np.float32)
    col = np.tile(np.arange(64), rows).astype(np.float32)
    inv = (10000.0 ** (-np.arange(0, 64, 2, dtype=np.float32) / 64)).astype(np.float32)
    ang = np.concatenate([row[:, None] * inv, col[:, None] * inv], axis=-1).astype(np.float32)
    return np.cos(ang).astype(np.float32), np.sin(ang).astype(np.float32)


def make_in_maps(inputs, cfg, n_cores):
    g = lambda k: np.asarray(inputs[k])
    SEGL, LH = cfg["SEGL"], cfg["LH"]
    xp = g("x_prompt")[0]
    xs = g("x_sample")
    cp, sp = _rope_tables(LH)
    cs, ss = _rope_tables(SEGL)
    cos_k = np.concatenate([cp, cs], 0); sin_k = np.concatenate([sp, ss], 0)
    shared = {
        "norm_mix": np.ascontiguousarray(g("norm_mix")[0]), "norm_ffn": np.ascontiguousarray(g("norm_ffn")[0]),
        "norm_final": np.ascontiguousarray(g("norm_final")),
        "w_in": np.ascontiguousarray(g("w_in")[0]), "q_norm": np.ascontiguousarray(g("q_norm")[0]),
        "k_norm": np.ascontiguousarray(g("k_norm")[0]), "cos_k": cos_k, "sin_k": sin_k,
        "ident": np.eye(128, dtype=np.float32),
        "w_glu": np.ascontiguousarray(g("w_glu")[0]), "w_up_attn": np.ascontiguousarray(g("w_up_attn")[0]),
        "w_up_ssm": np.ascontiguousarray(g("w_up_ssm")[0]), "w_out": np.ascontiguousarray(g("w_out")[0]),
        "peer_w_query": np.ascontiguousarray(g("peer_w_query")[0]),
        "peer_skT": np.ascontiguousarray(g("peer_sub_keys")[0].reshape(-1, 128, 128).transpose(2, 0, 1)),
        "peer_u": np.ascontiguousarray(g("peer_u")[0]), "peer_v": np.ascontiguousarray(g("peer_v")[0]),
    }
    shared.update(ssm_host_layout(g("ssm_A_re")[0], g("ssm_A_im")[0], g("ssm_log_dt")[0], g("ssm_B_re")[0],
                                  g("ssm_B_im")[0], g("ssm_C_re")[0], g("ssm_C_im")[0], g("ssm_D")[0]))
    in_maps = []
    for i in range(n_cores):
        sl = slice(SEGL * i, SEGL * (i + 1))
        m = dict(shared)
        m["x_own"] = np.ascontiguousarray(np.concatenate([xp[sl], xs[i]], axis=0))
        m["x_kv"] = np.ascontiguousarray(np.concatenate([xp, xs[i]], axis=0))
        m["cos_q"] = np.ascontiguousarray(np.concatenate([cp[sl], cs], 0))
        m["sin_q"] = np.ascontiguousarray(np.concatenate([sp[sl], ss], 0))
        oh = np.zeros(8, np.float32); oh[i] = 1.0
        m["onehot"] = oh
        in_maps.append(m)
    return in_maps


def kernel(**inputs):
    cfg = FULL
    nc = build_program(cfg)
    in_maps = make_in_maps(inputs, cfg, 8)
    res = run_bass_kernel_spmd(nc, in_maps, core_ids=list(range(8)))
    S = cfg["SEGL"]
    yp = np.concatenate([r["y_out"][:S] for r in res.results], axis=0)[None]
    ys = np.stack([r["y_out"][S:] for r in res.results], axis=0)
    return (yp.astype(np.float32), ys.astype(np.float32))
```

```python
import numpy as np
import ml_dtypes
import concourse.bass as bass
import concourse.mybir as mybir
from concourse.bass_utils import run_bass_kernel_spmd

F32 = mybir.dt.float32
BF16 = mybir.dt.bfloat16
ALU = mybir.AluOpType
ACT = mybir.ActivationFunctionType
AX = mybir.AxisListType

EPS = 1e-6


class Prog:
    ENGS = ("sync", "scalar", "gpsimd", "tensor", "vector")

    def __init__(self, nc):
        self.nc = nc
        self.q = {e: [] for e in self.ENGS}
        self.cum = 0
        self.barrier = 0
        self.in_par = False
        self.deferred = []

    def emit(self, eng, fn, dma=False):
        inc = 16 if dma else 1
        self.q[eng].append((fn, self.barrier, inc))
        self.cum += inc
        if not self.in_par:
            self.barrier = self.cum

    class _Par:
        def __init__(self, p):
            self.p = p

        def __enter__(self):
            self.p.in_par = True

        def __exit__(self, *a):
            self.p.in_par = False
            self.p.barrier = self.p.cum

    def par(self):
        return Prog._Par(self)

    def dma(self, out, in_, eng="sync", **kw):
        self.emit(eng, lambda e: e.dma_start(out=out, in_=in_, **kw), dma=True)

    def dma_cast(self, out, in_, **kw):
        self.emit("gpsimd", lambda e: e.dma_start(out=out, in_=in_, **kw), dma=True)

    def op(self, eng, name, *args, **kw):
        self.emit(eng, lambda e: getattr(e, name)(*args, **kw))

    def v(self, name, *args, **kw):
        self.op("vector", name, *args, **kw)

    def a(self, name, *args, **kw):
        self.op("scalar", name, *args, **kw)

    def g(self, name, *args, **kw):
        self.op("gpsimd", name, *args, **kw)

    def pe(self, name, *args, **kw):
        self.op("tensor", name, *args, **kw)

    def dma_deferred(self, out, in_):
        self.deferred.append((out, in_))

    def flush_deferred(self):
        for out, in_ in self.deferred:
            self.dma(out, in_)
        self.deferred = []

    def finish(self, G):
        self.flush_deferred()
        nc = self.nc
        total = self.cum
        with nc.Block() as block:
            def run(engname):
                def body(e):
                    last = -1
                    for fn, w, inc in self.q[engname]:
                        if w > last and w > 0:
                            e.wait_ge(G, w)
                            last = w
                        fn(e).then_inc(G, inc)
                    e.wait_ge(G, total)
                return body
            block.sync(run("sync"))
            block.scalar(run("scalar"))
            block.gpsimd(run("gpsimd"))
            block.tensor(run("tensor"))
            block.vector(run("vector"))


def stage_norm_T(p, nc, ctx, x_src, g_src, hT_dst, N, D, ident):
    KC = D // 128
    TB = next(t for t in (512, 256, 128) if N % t == 0)
    gb = ctx.enter_context(nc.sbuf_tensor(nc.make_name("gb", True), [128, D], F32))
    xt = ctx.enter_context(nc.sbuf_tensor(nc.make_name("xt", True), [128, D], F32))
    xn = ctx.enter_context(nc.sbuf_tensor(nc.make_name("xn", True), [128, D], BF16))
    ss = ctx.enter_context(nc.sbuf_tensor(nc.make_name("ss", True), [128, 2], F32))
    hb = ctx.enter_context(nc.sbuf_tensor(nc.make_name("hb", True), [128, KC, TB], BF16))
    pt = ctx.enter_context(nc.psum_tensor(nc.make_name("ptn", True), [128, 8, 128], BF16))
    p.dma(gb[:], g_src.partition_broadcast(128))
    for b0 in range(0, N, TB):
        for ti in range(TB // 128):
            t0 = b0 + ti * 128
            p.dma(xt[:], x_src[t0:t0 + 128, :])
            p.a("activation", xn[:], xt[:], ACT.Square, accum_out=ss[:, 0:1])
            p.a("activation", ss[:, 1:2], ss[:, 0:1], ACT.Sqrt, bias=EPS, scale=1.0 / D)
            p.v("reciprocal", ss[:, 1:2], ss[:, 1:2])
            p.v("scalar_tensor_tensor", xn[:], xt[:], ss[:, 1:2], gb[:], ALU.mult, ALU.mult)
            for c0 in range(0, KC, 8):
                nb = min(8, KC - c0)
                with p.par():
                    for c in range(nb):
                        p.pe("transpose", pt[:, c, :], xn[:, (c0 + c) * 128:(c0 + c + 1) * 128], ident[:])
                p.v("tensor_copy", hb[:, c0:c0 + nb, ti * 128:(ti + 1) * 128], pt[:, 0:nb, :])
        p.dma(hT_dst[:, :, b0:b0 + TB].rearrange("k d n -> d k n"), hb[:])


def stage_gemm(p, nc, ctx, actT, w_src, N, K, M, orient, epi, tag, w_bf16=False, pre=None):
    KC = K // 128
    TB = min(1024, N)
    ab = ctx.enter_context(nc.sbuf_tensor(nc.make_name("ab" + tag, True), [128, KC, TB], BF16))
    wts = [ctx.enter_context(nc.sbuf_tensor(nc.make_name("wt%d" % i + tag, True), [128, KC, 512], BF16)) for i in range(2)]
    ps = ctx.enter_context(nc.psum_tensor(nc.make_name("ps" + tag, True), [128, 512], F32))
    units = [(tb, mb) for tb in range(0, N, TB) for mb in range(0, M, 512)]

    def wload(u, k0, k1):
        tb, mb = units[u]
        mw = min(512, M - mb)
        wsrc_ap = w_src[k0 * 128:k1 * 128, mb:mb + mw].rearrange("(k d) m -> d k m", d=128)
        (p.dma if w_bf16 else p.dma_cast)(wts[u % 2][:, k0:k1, 0:mw], wsrc_ap)

    with p.par():
        for k0 in range(0, KC, 8):
            wload(0, k0, min(KC, k0 + 8))
    for u, (tb, mb) in enumerate(units):
        wt = wts[u % 2]
        mw = min(512, M - mb)
        if mb == 0:
            with p.par():
                for k0 in range(0, KC, 8):
                    k1 = min(KC, k0 + 8)
                    p.dma(ab[:, k0:k1, :], actT[k0:k1, :, tb:tb + TB].rearrange("k d n -> d k n"))
        if orient == "T":
            groups = [("T", tt) for tt in range(0, TB, 128)]
        else:
            groups = [("F", ms, ts) for ms in range(0, mw, 128) for ts in range(0, TB, 512)]
        ng = len(groups)
        cuts = [(KC * gi) // ng for gi in range(ng + 1)]
        for gi, g in enumerate(groups):
            with p.par():
                p.flush_deferred()
                if pre is not None:
                    if g[0] == "T":
                        pre(tb + g[1], 128, mb, mw)
                    else:
                        pre(tb + g[2], min(512, TB - g[2]), mb + g[1], 128)
                if u + 1 < len(units) and cuts[gi + 1] > cuts[gi]:
                    wload(u + 1, cuts[gi], cuts[gi + 1])
                if g[0] == "T":
                    tt = g[1]
                    for kc in range(KC):
                        p.pe("matmul", ps[:, 0:mw], ab[:, kc, tt:tt + 128], wt[:, kc, 0:mw],
                             start=(kc == 0), stop=(kc == KC - 1))
                else:
                    ms, ts = g[1], g[2]
                    tw = min(512, TB - ts)
                    for kc in range(KC):
                        p.pe("matmul", ps[:, 0:tw], wt[:, kc, ms:ms + 128], ab[:, kc, ts:ts + tw],
                             start=(kc == 0), stop=(kc == KC - 1))
            if g[0] == "T":
                epi(ps[:, 0:mw], tb + g[1], 128, mb, mw)
            else:
                tw = min(512, TB - g[2])
                epi(ps[:, 0:tw], tb + g[2], tw, mb + g[1], 128)
    p.flush_deferred()


def apv(t, off, dims):
    ps = 1
    for d in t.shape[1:]:
        ps *= d
    return bass.AP(t, off, [[ps, 128]] + [list(d) for d in dims])


class QKEpi:
    def __init__(self, p, nc, ctx, gvec_src, cos_src, sin_src, dstT, ident, tag):
        self.p, self.nc, self.dstT, self.ident = p, nc, dstT, ident
        self.cos_src, self.sin_src = cos_src, sin_src
        mk = lambda n, sh, dt: ctx.enter_context(nc.sbuf_tensor(nc.make_name(n + tag, True), sh, dt))
        self.t32 = mk("qe_t", [128, 512], F32)
        self.sq = mk("qe_s", [128, 512], F32)
        self.sq2 = mk("qe_s2", [128, 512], F32)
        self.ssh = mk("qe_h", [128, 8], F32)
        self.gqb = mk("qe_g", [128, 128], F32)
        self.cs = mk("qe_c", [128, 2, 64], F32)
        self.rb = mk("qe_r", [128, 512], BF16)
        self.qTs = mk("qe_q", [128, 4, 128], BF16)
        self.ptq = ctx.enter_context(nc.psum_tensor(nc.make_name("qe_p" + tag, True), [128, 4, 128], BF16))
        p.dma(self.gqb[:], gvec_src.partition_broadcast(128))
        self.cur_tok = None

    def __call__(self, ps, tok0, ntok, m0, nm):
        p = self.p
        nh = nm // 128
        t32, sq, sq2, ssh, rb = self.t32, self.sq, self.sq2, self.ssh, self.rb
        if self.cur_tok != tok0:
            with p.par():
                p.dma(self.cs[:, 0, :], self.cos_src[tok0:tok0 + 128, :])
                p.dma(self.cs[:, 1, :], self.sin_src[tok0:tok0 + 128, :])
            self.cur_tok = tok0
        p.v("tensor_copy", t32[:, 0:nm], ps)
        p.v("tensor_tensor", sq[:, 0:nm], t32[:, 0:nm], t32[:, 0:nm], ALU.mult)
        p.v("tensor_reduce", ssh[:, 0:nh], apv(sq, 0, [[128, nh], [1, 128]]), AX.X, ALU.add)
        p.a("activation", ssh[:, 4:4 + nh], ssh[:, 0:nh], ACT.Sqrt, bias=EPS, scale=1.0 / 128)
        p.v("reciprocal", ssh[:, 4:4 + nh], ssh[:, 4:4 + nh])
        v3 = lambda t: apv(t, 0, [[128, nh], [1, 128]])
        p.v("tensor_tensor", v3(t32), v3(t32), apv(ssh, 4, [[1, nh], [0, 128]]), ALU.mult)
        p.v("tensor_tensor", v3(t32), v3(t32), apv(self.gqb, 0, [[0, nh], [1, 128]]), ALU.mult)
        x = lambda t, half: apv(t, half * 32, [[128, nh], [64, 2], [1, 32]])
        cst = lambda which: apv(self.cs, which * 64, [[0, nh], [32, 2], [1, 32]])
        p.v("tensor_tensor", x(sq, 0), x(t32, 0), cst(0), ALU.mult)
        p.v("tensor_tensor", x(sq, 1), x(t32, 1), cst(1), ALU.mult)
        p.v("tensor_tensor", x(rb, 0), x(sq, 0), x(sq, 1), ALU.subtract)
        p.v("tensor_tensor", x(sq2, 0), x(t32, 0), cst(1), ALU.mult)
        p.v("tensor_tensor", x(sq2, 1), x(t32, 1), cst(0), ALU.mult)
        p.v("tensor_tensor", x(rb, 1), x(sq2, 0), x(sq2, 1), ALU.add)
        with p.par():
            for h in range(nh):
                p.pe("transpose", self.ptq[:, h, :], rb[:, h * 128:(h + 1) * 128], self.ident[:])
        p.v("tensor_copy", self.qTs[:, 0:nh, :], self.ptq[:, 0:nh, :])
        h0 = m0 // 128
        p.dma(self.dstT[h0:h0 + nh, :, tok0:tok0 + 128].rearrange("h d n -> d h n"), self.qTs[:, 0:nh, :])


def stage_attention(p, nc, ctx, qT_src, kT_src, v_src, attnT_dst, gq_src, gk_src, segs, NH, NKV, tag):
    GQ = NH // NKV
    NKmax = max(s[3] for s in segs)
    NQmax = max(s[1] for s in segs)
    mk = lambda n, sh, dt: ctx.enter_context(nc.sbuf_tensor(nc.make_name(n + tag, True), sh, dt))
    kT = mk("at_k", [128, NKmax], BF16)
    vv = mk("at_v", [128, NKmax // 128, 128], BF16)
    qTt = mk("at_q", [128, NQmax], BF16)
    pT = mk("at_p", [128, 4, 512], BF16)
    ones = mk("at_1", [128, 128], BF16)
    rc = mk("at_rc", [128, 512], F32)
    obb = mk("at_ob", [128, 512], BF16)
    gg = mk("at_g", [128, 2, 128], F32)
    cb = mk("at_cb", [128, 4], F32)
    sc = ctx.enter_context(nc.psum_tensor(nc.make_name("at_sc" + tag, True), [128, 4, 512], F32))
    acc_o = ctx.enter_context(nc.psum_tensor(nc.make_name("at_ao" + tag, True), [128, 512], F32))
    acc_s = ctx.enter_context(nc.psum_tensor(nc.make_name("at_as" + tag, True), [128, 512], F32))
    scale = 128 ** -0.5
    p.v("memset", ones[:], 1.0)
    with p.par():
        p.dma(gg[:, 0, :], gq_src.partition_broadcast(128))
        p.dma(gg[:, 1, :], gk_src.partition_broadcast(128))
    p.v("tensor_reduce", cb[:, 0:2], gg[:], AX.X, ALU.max, apply_absolute_value=True)
    p.v("tensor_tensor", cb[:, 2:3], cb[:, 0:1], cb[:, 1:2], ALU.mult)
    p.v("tensor_scalar", cb[:, 3:4], cb[:, 2:3], -(128 ** 0.5), None, ALU.mult)
    for (qoff, NQ, koff, NK) in segs:
        NKC = NK // 128
        NG = NKC // 2
        for kvh in range(NKV):
            with p.par():
                p.dma(kT[:, 0:NK], kT_src[kvh, :, koff:koff + NK])
                p.dma(vv[:, 0:NKC, :], v_src[koff:koff + NK, kvh * 128:(kvh + 1) * 128].rearrange("(c p) d -> p c d", p=128))
            for qh in range(GQ):
                head = kvh * GQ + qh
                p.dma(qTt[:, 0:NQ], qT_src[head, :, qoff:qoff + NQ])
                for qb in range(0, NQ, 512):
                    qw = min(512, NQ - qb)
                    for n in range(-1, NG + 1):
                        with p.par():
                            if 0 <= n - 1:
                                m = n - 1
                                for j in range(2):
                                    kc = 2 * m + j
                                    first = (kc == 0); last = (kc == NKC - 1)
                                    p.pe("matmul", acc_o[:, 0:qw], vv[:, kc, :], pT[:, 2 * (m % 2) + j, 0:qw], start=first, stop=last)
                                    p.pe("matmul", acc_s[:, 0:qw], ones[:], pT[:, 2 * (m % 2) + j, 0:qw], start=first, stop=last)
                            if n + 1 < NG:
                                m = n + 1
                                for j in range(2):
                                    kc = 2 * m + j
                                    p.pe("matmul", sc[:, 2 * (m % 2) + j, 0:qw], kT[:, kc * 128:(kc + 1) * 128],
                                         qTt[:, qb:qb + qw], start=True, stop=True)
                            if 0 <= n < NG:
                                b = 2 * (n % 2)
                                p.a("activation", pT[:, b:b + 2, 0:qw], sc[:, b:b + 2, 0:qw], ACT.Exp, bias=cb[:, 3:4], scale=scale)
                    p.v("reciprocal", rc[:, 0:qw], acc_s[:, 0:qw])
                    p.v("tensor_tensor", obb[:, 0:qw], acc_o[:, 0:qw], rc[:, 0:qw], ALU.mult)
                    p.dma(attnT_dst[head, :, qoff + qb:qoff + qb + qw], obb[:, 0:qw])


import math


def ssm_host_layout(A_re, A_im, log_dt, B_re, B_im, C_re, C_im, D):
    _, H, P = A_re.shape
    Gs = B_re.shape[-1]
    NP = H // 2
    NJ = H * Gs // 128
    lay = lambda a: np.ascontiguousarray(a.reshape(2, NP, 2 * P).transpose(2, 0, 1))
    dte = np.ascontiguousarray(np.repeat(log_dt.reshape(2, NP, 2, 1), P, axis=3).reshape(2, NP, 2 * P).transpose(2, 0, 1))
    Bt = np.zeros((128, 2, 2, NJ, 128), np.float32)
    Bt3 = np.zeros((128, 2, 2, NJ, 128), np.float32)
    Ct = np.zeros((128, 2, 2, NP, 64), np.float32)
    for ri, (Bx, Cx) in enumerate(((B_re, C_re), (B_im, C_im))):
        for pair in range(NP):
            j, q = pair // 4, pair % 4
            for h2 in range(2):
                h = 2 * pair + h2
                blk = Bx[:, h].transpose(2, 0, 1)
                r0 = 32 * q + 16 * h2
                (Bt if q % 2 == 0 else Bt3)[r0:r0 + Gs, :, ri, j, h2 * P:(h2 + 1) * P] = blk
                c0 = (q % 2) * 32 + 16 * h2
                Ct[h2 * P:(h2 + 1) * P, :, ri, pair, c0:c0 + Gs] = Cx[:, h].transpose(2, 0, 1)
    Dt = np.ascontiguousarray(D.reshape(NJ, 128).T)
    return dict(ssm_Ar=lay(A_re), ssm_Ai=lay(A_im), ssm_dt=dte, ssm_Bt=Bt, ssm_Bt3=Bt3, ssm_Ct=Ct, ssm_Dt=Dt)


def stage_ssm(p, nc, ctx, uT_hist, uT_own, zT_own, y_scr, prm, oh_src, LH, SEGL, seqs, NP, NJ, tag=""):
    from contextlib import ExitStack
    TBK = 64
    NSEG = LH // SEGL
    PPU = min(32, NP)
    mk = lambda n, sh, dt, c=ctx: c.enter_context(nc.sbuf_tensor(nc.make_name("ss_" + n + tag, True), sh, dt))
    A2 = mk("A2", [128, 2, 2, NP], F32); B2 = mk("B2", [128, 2, 2, NP], F32)
    Bt = mk("Bt", [128, 2, 2, NJ, 128], BF16); Bt3 = mk("Bt3", [128, 2, 2, NJ, 128], BF16)
    Cb = mk("Cb", [128, 2, 2, NP, 64], BF16)
    Dt = mk("Dt", [128, NJ], F32)
    oh = mk("oh", [128, 8], F32)
    Xc = mk("Xc", [128, 2, 2, NP], F32); Xin = mk("Xin", [128, 2, 2, NP], F32)
    m1 = mk("m1", [128, 2, 2, NP], F32); m2 = mk("m2", [128, 2, 2, NP], F32)
    with ExitStack() as c2:
        mk2 = lambda n, sh, dt: mk(n, sh, dt, c2)
        Ar = mk2("ar", [128, 2, NP], F32); Ai = mk2("ai", [128, 2, NP], F32); dtv = mk2("dt", [128, 2, NP], F32)
        t1 = mk2("t1", [128, 2, NP], F32); t2 = mk2("t2", [128, 2, NP], F32); t3 = mk2("t3", [128, 2, NP], F32)
        cc = mk2("cc", [128, 2, NP], F32); sn = mk2("sn", [128, 2, NP], F32)
        lbr = mk2("lbr", [128, 2, NP], F32); lbi = mk2("lbi", [128, 2, NP], F32)
        cr = mk2("cr", [128, 2, NP], F32); ci = mk2("ci", [128, 2, NP], F32)
        Ctf = mk2("Ctf", [128, 2, NP, 64], F32)
        W1 = mk2("W1", [128, NP, 64], F32); W2 = mk2("W2", [128, NP, 64], F32)
        with p.par():
            p.dma(Ar[:], prm["Ar"]); p.dma(Ai[:], prm["Ai"]); p.dma(dtv[:], prm["dt"])
            p.dma(Dt[:], prm["Dt"]); p.dma(oh[:], oh_src.partition_broadcast(128))
        for d in range(2):
            p.dma_cast(Bt[:, d], prm["Bt"][:, d])
            p.dma_cast(Bt3[:, d], prm["Bt3"][:, d])
        p.a("activation", dtv[:], dtv[:], ACT.Exp)
        p.v("tensor_tensor", t1[:], Ar[:], dtv[:], ALU.mult)
        p.v("tensor_tensor", t2[:], Ai[:], dtv[:], ALU.mult)
        p.a("activation", lbr[:], t1[:], ACT.Exp)
        p.a("activation", sn[:], t2[:], ACT.Sin, scale=1.0 / 32)
        p.v("tensor_scalar", t3[:], t2[:], 1.0 / 32, math.pi / 2, ALU.mult, ALU.add)
        p.a("activation", cc[:], t3[:], ACT.Sin)
        for _ in range(5):
            p.v("tensor_tensor", t1[:], cc[:], cc[:], ALU.mult)
            p.v("tensor_tensor", t2[:], sn[:], sn[:], ALU.mult)
            p.v("tensor_tensor", t3[:], cc[:], sn[:], ALU.mult)
            p.v("tensor_tensor", cc[:], t1[:], t2[:], ALU.subtract)
            p.v("tensor_scalar", sn[:], t3[:], 2.0, None, ALU.mult)
        p.v("tensor_tensor", lbi[:], lbr[:], sn[:], ALU.mult)
        p.v("tensor_tensor", lbr[:], lbr[:], cc[:], ALU.mult)
        for ri in range(2):
            p.v("tensor_copy", A2[:, :, ri, :], lbr[:])
        p.v("tensor_scalar", B2[:, :, 0, :], lbi[:], -1.0, None, ALU.mult)
        p.v("tensor_copy", B2[:, :, 1, :], lbi[:])
        p.v("tensor_scalar", t1[:], lbr[:], -1.0, None, ALU.add)
        p.v("tensor_tensor", t2[:], Ar[:], Ar[:], ALU.mult)
        p.v("tensor_tensor", t3[:], Ai[:], Ai[:], ALU.mult)
        p.v("tensor_tensor", t2[:], t2[:], t3[:], ALU.add)
        p.v("reciprocal", t2[:], t2[:])
        p.v("tensor_tensor", cr[:], t1[:], Ar[:], ALU.mult)
        p.v("tensor_tensor", t3[:], lbi[:], Ai[:], ALU.mult)
        p.v("tensor_tensor", cr[:], cr[:], t3[:], ALU.add)
        p.v("tensor_tensor", cr[:], cr[:], t2[:], ALU.mult)
        p.v("tensor_tensor", ci[:], lbi[:], Ar[:], ALU.mult)
        p.v("tensor_tensor", t3[:], t1[:], Ai[:], ALU.mult)
        p.v("tensor_tensor", ci[:], ci[:], t3[:], ALU.subtract)
        p.v("tensor_tensor", ci[:], ci[:], t2[:], ALU.mult)
        for d in range(2):
            p.dma(Ctf[:], prm["Ct"][:, d])
            bc = lambda t: apv(t, d * NP, [[1, NP], [0, 64]])
            p.v("tensor_tensor", W1[:], Ctf[:, 0], bc(cr), ALU.mult)
            p.v("tensor_tensor", W2[:], Ctf[:, 1], bc(ci), ALU.mult)
            p.v("tensor_tensor", Cb[:, d, 0], W1[:], W2[:], ALU.subtract)
            p.v("tensor_tensor", W1[:], Ctf[:, 0], bc(ci), ALU.mult)
            p.v("tensor_tensor", W2[:], Ctf[:, 1], bc(cr), ALU.mult)
            p.v("tensor_tensor", W1[:], W1[:], W2[:], ALU.add)
            p.v("tensor_scalar", Cb[:, d, 1], W1[:], -1.0, None, ALU.mult)
    XB = mk("XB", [128, 2, 2, NP, TBK], F32)
    Xbf = mk("Xbf", [128, 2, NP, TBK], BF16)
    uf = mk("uf", [128, NJ, TBK], BF16); ub = mk("ub", [128, NJ, TBK], BF16)
    yt = mk("yt", [128, NJ, TBK], F32); yp = mk("yp", [128, NJ, TBK], F32); zb = mk("zb", [128, NJ, TBK], BF16)
    psu = ctx.enter_context(nc.psum_tensor(nc.make_name("ss_psu" + tag, True), [128, PPU, TBK], F32))
    psy = ctx.enter_context(nc.psum_tensor(nc.make_name("ss_psy" + tag, True), [128, NJ, TBK], F32))
    RS = NP * TBK
    DS = 2 * RS
    PS = 2 * DS

    def slot(sf, sb):
        return bass.AP(XB, sf, [[PS, 128], [DS + sb - sf, 2], [RS, 2], [TBK, NP]])

    def slot_ri(sf, sb, ri):
        return bass.AP(XB, sf + ri * RS, [[PS, 128], [DS + sb - sf, 2], [TBK, NP]])

    def block_step(usrc, tf0, tb0):
        with p.par():
            p.dma(uf[:], usrc[:, :, tf0:tf0 + TBK].rearrange("j c t -> c j t"))
            p.dma(ub[:], usrc[:, :, tb0:tb0 + TBK].rearrange("j c t -> c j t"))
        for d in range(2):
            u = uf if d == 0 else ub
            for ri in range(2):
                for pb in range(0, NP, PPU):
                    jb, nj = pb // 4, PPU // 4
                    for hf in range(2):
                        with p.par():
                            for jj in range(nj):
                                j = jb + jj
                                for q in (2 * hf, 2 * hf + 1):
                                    tb_ = Bt if q % 2 == 0 else Bt3
                                    p.pe("matmul", psu[:, 4 * jj + q, :], tb_[64 * hf:64 * hf + 64, d, ri, j, :],
                                         u[64 * hf:64 * hf + 64, j, :], start=True, stop=True)
                    p.a("copy", XB[:, d, ri, pb:pb + PPU, :], psu[:, 0:PPU, :])
        with p.par():
            for s_ in range(TBK):
                prev = Xc[:] if s_ == 0 else slot(s_ - 1, TBK - s_)
                pr = (lambda r: Xc[:, :, r, :]) if s_ == 0 else (lambda r: slot_ri(s_ - 1, TBK - s_, r))
                cur = slot(s_, TBK - 1 - s_)
                p.v("tensor_tensor", m1[:], prev, A2[:], ALU.mult)
                p.v("tensor_tensor", m2[:, :, 0, :], pr(1), B2[:, :, 0, :], ALU.mult)
                p.v("tensor_tensor", m2[:, :, 1, :], pr(0), B2[:, :, 1, :], ALU.mult)
                p.v("tensor_tensor", m1[:], m1[:], m2[:], ALU.add)
                p.v("tensor_tensor", cur, m1[:], cur, ALU.add)
        p.v("tensor_copy", Xc[:], slot(TBK - 1, 0))

    p.v("memset", Xin[:], 0.0)
    p.v("memset", Xc[:], 0.0)
    if NSEG > 1:
        for n in range(LH // TBK):
            if (TBK * n) >= LH - SEGL:
                break
            block_step(uT_hist, TBK * n, LH - TBK * (n + 1))
            done = TBK * (n + 1)
            if done % SEGL == 0:
                k = done // SEGL
                p.v("scalar_tensor_tensor", Xin[:, 0], Xc[:, 0], oh[:, k:k + 1], Xin[:, 0], ALU.mult, ALU.add)
                kb = NSEG - 1 - k
                p.v("scalar_tensor_tensor", Xin[:, 1], Xc[:, 1], oh[:, kb:kb + 1], Xin[:, 1], ALU.mult, ALU.add)
    NB = SEGL // TBK
    for (o, use_hist) in seqs:
        if use_hist:
            p.v("tensor_copy", Xc[:], Xin[:])
        else:
            p.v("memset", Xc[:], 0.0)
        for n in range(NB):
            tf0 = o + TBK * n
            tb0 = o + SEGL - TBK * (n + 1)
            block_step(uT_own, tf0, tb0)
            for d in range(2):
                tok0 = tf0 if d == 0 else tb0
                u = uf if d == 0 else ub
                p.a("copy", Xbf[:], XB[:, d])
                for half in range(2):
                    with p.par():
                        for j in range(NJ):
                            for qq in range(2):
                                pair = 4 * j + 2 * half + qq
                                for ri in range(2):
                                    p.pe("matmul", psy[64 * half:64 * half + 64, j, :], Cb[:, d, ri, pair, :],
                                         Xbf[:, ri, pair, :], start=(qq == 0 and ri == 0), stop=(qq == 1 and ri == 1))
                dst = y_scr[:, :, tok0:tok0 + TBK].rearrange("j c t -> c j t")
                if n < NB // 2:
                    p.v("tensor_copy", yt[:], psy[:])
                    p.dma(dst, yt[:])
                else:
                    p.dma(yp[:], dst)
                    p.v("tensor_tensor", yt[:], psy[:], yp[:], ALU.add)
                    p.v("tensor_tensor", yp[:], u[:], apv(Dt, 0, [[1, NJ], [0, TBK]]), ALU.mult)
                    p.v("tensor_tensor", yt[:], yt[:], yp[:], ALU.add)
                    p.a("activation", zb[:], yt[:], ACT.Gelu_apprx_tanh)
                    p.dma(zT_own[:, :, tok0:tok0 + TBK].rearrange("j c t -> c j t"), zb[:])


def stage_gemm_bigk(p, nc, ctx, actT, w_src, N, K, M, epi, tag, pre=None):
    KC = K // 128
    KP = min(16, KC)
    TB = min(1024, N)
    NT = TB // 128
    abs_ = [ctx.enter_context(nc.sbuf_tensor(nc.make_name("bk_a%d" % i + tag, True), [128, KP, TB], BF16)) for i in range(2)]
    wts = [ctx.enter_context(nc.sbuf_tensor(nc.make_name("bk_w%d" % i + tag, True), [128, KP, 512], BF16)) for i in range(2)]
    ps = ctx.enter_context(nc.psum_tensor(nc.make_name("bk_p" + tag, True), [128, NT, 512], F32))
    units = [(tb, mb, k0) for tb in range(0, N, TB) for mb in range(0, M, 512) for k0 in range(0, KC, KP)]

    def loads(u):
        tb, mb, k0 = units[u]
        mw = min(512, M - mb)
        for kk in range(0, KP, 8):
            p.dma(abs_[u % 2][:, kk:kk + 8, :], actT[k0 + kk:k0 + kk + 8, :, tb:tb + TB].rearrange("k d n -> d k n"))
            p.dma_cast(wts[u % 2][:, kk:kk + 8, 0:mw],
                       w_src[(k0 + kk) * 128:(k0 + kk + 8) * 128, mb:mb + mw].rearrange("(k d) m -> d k m", d=128))

    with p.par():
        loads(0)
    for u, (tb, mb, k0) in enumerate(units):
        ab, wt = abs_[u % 2], wts[u % 2]
        mw = min(512, M - mb)
        with p.par():
            p.flush_deferred()
            if pre is not None:
                npc = (KC + KP - 1) // KP
                kpi = k0 // KP
                for t in range(NT):
                    if (t * npc) // NT == kpi:
                        pre(t, tb + t * 128, 128, mb, mw)
            if u + 1 < len(units):
                loads(u + 1)
            for t in range(NT):
                for kc in range(KP):
                    p.pe("matmul", ps[:, t, 0:mw], ab[:, kc, t * 128:(t + 1) * 128], wt[:, kc, 0:mw],
                         start=(k0 == 0 and kc == 0), stop=(k0 + kc == KC - 1))
        if k0 + KP >= KC:
            for t in range(NT):
                epi(ps[:, t, 0:mw], tb + t * 128, 128, mb, mw, t)
                if t < NT - 1:
                    p.flush_deferred()
    p.flush_deferred()


def stage_peer_ut(p, nc, ctx, U_src, UT_d, NE, D, ident):
    KC = D // 128
    ur = ctx.enter_context(nc.sbuf_tensor("pu_r", [128, D], BF16))
    us = ctx.enter_context(nc.sbuf_tensor("pu_s", [128, KC, 512], BF16))
    pt = ctx.enter_context(nc.psum_tensor("pu_p", [128, 8, 128], BF16))
    for e0 in range(0, NE, 512):
        for eb in range(4):
            p.dma_cast(ur[:], U_src[e0 + eb * 128:e0 + (eb + 1) * 128, :])
            for c0 in range(0, KC, 8):
                nb = min(8, KC - c0)
                with p.par():
                    for c in range(nb):
                        p.pe("transpose", pt[:, c, :], ur[:, (c0 + c) * 128:(c0 + c + 1) * 128], ident[:])
                p.v("tensor_copy", us[:, c0:c0 + nb, eb * 128:(eb + 1) * 128], pt[:, 0:nb, :])
        p.dma(UT_d[:, e0:e0 + 512].rearrange("(k d) e -> d k e", d=128), us[:])


def stage_peer_gate(p, nc, ctx, qT_d, skT_src, G_d, N, PH, tag=""):
    HC = 2 * PH
    mk = lambda n, sh, dt: ctx.enter_context(nc.sbuf_tensor(nc.make_name("pg_" + n + tag, True), sh, dt))
    skT = mk("sk", [128, HC, 128], BF16)
    qt = mk("q", [128, HC, 128], BF16)
    S = mk("S", [128, HC, 128], F32)
    wk = mk("wk", [128, 256], F32)
    top1 = mk("t1", [128, HC, 16], F32)
    cand = mk("cd", [128, PH, 256], F32)
    top2 = mk("t2", [128, PH, 16], F32)
    st = mk("st", [128, 4, PH], F32)
    ex = mk("ex", [128, PH, 16], F32)
    HB = min(4, PH)
    T1 = mk("T1", [128, HB, 16, 128], F32)
    Mk = mk("Mk", [128, HB, 16, 128], F32)
    Gf = mk("Gf", [128, 16, 128], F32)
    Gb = mk("Gb", [128, 128 * 128], BF16)
    ps = ctx.enter_context(nc.psum_tensor(nc.make_name("pg_ps" + tag, True), [128, HC, 128], F32))
    p.dma_cast(skT[:], skT_src)
    for t0 in range(0, N, 128):
        p.dma(qt[:], qT_d[:, :, t0:t0 + 128].rearrange("c q n -> q c n"))
        with p.par():
            for hc in range(HC):
                p.pe("matmul", ps[:, hc, :], qt[:, hc, :], skT[:, hc, :], start=True, stop=True)
        p.v("tensor_copy", S[:], ps[:])
        for hc in range(HC):
            p.v("max", out=top1[:, hc, 0:8], in_=S[:, hc, :])
            p.v("match_replace", out=wk[:, 0:128], in_to_replace=top1[:, hc, 0:8], in_values=S[:, hc, :], imm_value=-1e30)
            p.v("max", out=top1[:, hc, 8:16], in_=wk[:, 0:128])
        p.v("tensor_tensor", apv(cand, 0, [[256, PH], [16, 16], [1, 16]]), apv(top1, 0, [[32, PH], [1, 16], [0, 16]]),
            apv(top1, 16, [[32, PH], [0, 16], [1, 16]]), ALU.add)
        for h in range(PH):
            p.v("max", out=top2[:, h, 0:8], in_=cand[:, h, :])
            p.v("match_replace", out=wk[:], in_to_replace=top2[:, h, 0:8], in_values=cand[:, h, :], imm_value=-1e30)
            p.v("max", out=top2[:, h, 8:16], in_=wk[:])
        p.v("tensor_copy", st[:, 0, :], top2[:, :, 15])
        p.v("tensor_tensor", ex[:], top2[:], apv(top2, 0, [[16, PH], [0, 16]]), ALU.subtract)
        p.a("activation", ex[:], ex[:], ACT.Exp)
        p.v("tensor_reduce", st[:, 2, :], ex[:], AX.X, ALU.add)
        p.a("activation", st[:, 3, :], st[:, 2, :], ACT.Ln)
        p.v("tensor_tensor", st[:, 1, :], st[:, 3, :], top2[:, :, 0], ALU.add)
        p.v("tensor_scalar", st[:, 1, :], st[:, 1, :], -1.0, None, ALU.mult)
        for g0 in range(0, 128, 16):
            for hb in range(0, PH, HB):
                hs = list(range(hb, min(PH, hb + HB)))
                with p.par():
                    for h in hs:
                        p.v("tensor_tensor", Mk[:, h - hb], apv(S, 2 * h * 128 + g0, [[1, 16], [0, 128]]),
                            apv(S, (2 * h + 1) * 128, [[0, 16], [1, 128]]), ALU.add)
                with p.par():
                    for h in hs:
                        p.a("activation", T1[:, h - hb], Mk[:, h - hb], ACT.Exp, bias=st[:, 1, h:h + 1])
                with p.par():
                    for h in hs:
                        if h == 0:
                            p.v("scalar_tensor_tensor", Gf[:], Mk[:, 0], st[:, 0, 0:1], T1[:, 0], ALU.is_ge, ALU.mult)
                        else:
                            p.v("scalar_tensor_tensor", T1[:, h - hb], Mk[:, h - hb], st[:, 0, h:h + 1], T1[:, h - hb],
                                ALU.is_ge, ALU.mult)
                            p.v("tensor_tensor", Gf[:], Gf[:], T1[:, h - hb], ALU.add)
            p.a("copy", Gb[:, g0 * 128:(g0 + 16) * 128], Gf[:].rearrange("p a b -> p (a b)"))
        with p.par():
            for c in range(0, 128 * 128, 4096):
                p.dma(G_d[t0:t0 + 128, c:c + 4096], Gb[:, c:c + 4096])


def stage_norm_tok(p, nc, ctx, x_src, g_src, y_dst, N, D):
    gb = ctx.enter_context(nc.sbuf_tensor(nc.make_name("fgb", True), [128, D], F32))
    xt = ctx.enter_context(nc.sbuf_tensor(nc.make_name("fxt", True), [128, D], F32))
    xn = ctx.enter_context(nc.sbuf_tensor(nc.make_name("fxn", True), [128, D], F32))
    ss = ctx.enter_context(nc.sbuf_tensor(nc.make_name("fss", True), [128, 2], F32))
    p.dma(gb[:], g_src.partition_broadcast(128))
    for t0 in range(0, N, 128):
        p.dma(xt[:], x_src[t0:t0 + 128, :])
        p.a("activation", xn[:], xt[:], ACT.Square, accum_out=ss[:, 0:1])
        p.a("activation", ss[:, 1:2], ss[:, 0:1], ACT.Sqrt, bias=EPS, scale=1.0 / D)
        p.v("reciprocal", ss[:, 1:2], ss[:, 1:2])
        p.v("scalar_tensor_tensor", xn[:], xt[:], ss[:, 1:2], gb[:], ALU.mult, ALU.mult)
        p.dma(y_dst[t0:t0 + 128, :], xn[:])


def build_program(cfg):
    from contextlib import ExitStack
    D = cfg["D"]; NOWN = cfg["NOWN"]; SEGL = cfg["SEGL"]; LH = cfg["LH"]; NKV = LH + SEGL
    NH = cfg["NH"]; NKVH = cfg["NKVH"]; SW = cfg["SW"]; PH = cfg["PH"]; NE = cfg["NE"]
    NP = SW // 32; NJ = SW // 128
    AW = NH * 128; KW = NKVH * 128
    INW = AW + 2 * KW + SW + 2 * D
    cU = AW + 2 * KW; cG = cU + SW
    DC = D // 128
    nc = bass.Bass("TRN2", target_bir_lowering=False)
    din = lambda n, sh: nc.dram_tensor(n, sh, F32, kind="ExternalInput").ap()
    x_own = din("x_own", [NOWN, D]); x_kv = din("x_kv", [NKV, D])
    norm_mix = din("norm_mix", [D]); norm_ffn = din("norm_ffn", [D]); norm_final = din("norm_final", [D])
    w_in = din("w_in", [D, INW]); gq = din("q_norm", [128]); gk = din("k_norm", [128])
    cos_q = din("cos_q", [NOWN, 64]); sin_q = din("sin_q", [NOWN, 64])
    cos_k = din("cos_k", [NKV, 64]); sin_k = din("sin_k", [NKV, 64])
    ident_in = din("ident", [128, 128]); oh_src = din("onehot", [8])
    prm = dict(Ar=din("ssm_Ar", [128, 2, NP]), Ai=din("ssm_Ai", [128, 2, NP]), dt=din("ssm_dt", [128, 2, NP]),
               Bt=din("ssm_Bt", [128, 2, 2, NJ, 128]), Bt3=din("ssm_Bt3", [128, 2, 2, NJ, 128]),
               Ct=din("ssm_Ct", [128, 2, 2, NP, 64]), Dt=din("ssm_Dt", [128, NJ]))
    w_glu = din("w_glu", [SW, SW]); w_up_attn = din("w_up_attn", [AW, D]); w_up_ssm = din("w_up_ssm", [SW, D])
    w_out = din("w_out", [D, D]); w_query = din("peer_w_query", [D, PH * 256])
    skT_src = din("peer_skT", [128, 2 * PH, 128]); U_src = din("peer_u", [NE, D]); V_src = din("peer_v", [NE, D])
    dbg = cfg.get("debug", False)
    y_out = nc.dram_tensor("y_out", [NOWN, D], F32, kind="ExternalOutput").ap()

    def scr(n, sh, dt=BF16):
        return nc.dram_tensor(n, sh, dt, kind=("ExternalOutput" if dbg else "Internal")).ap()
    hT_own = scr("hT_own", [DC, 128, NOWN]); hT_kv = scr("hT_kv", [DC, 128, NKV])
    qT = scr("qT", [NH, 128, NOWN]); kT = scr("kT", [NKVH, 128, NKV]); vt = scr("vt", [NKV, KW])
    attnT = scr("attnT", [NH, 128, NOWN])
    uT_hist = scr("uT_hist", [NJ, 128, LH]); uT_own = scr("uT_own", [NJ, 128, NOWN])
    zT = scr("zT", [NJ, 128, NOWN]); y_scr = scr("y_scr", [NJ, 128, NOWN], F32)
    soT = scr("soT", [NJ, 128, NOWN]); sgT = scr("sgT", [2 * DC, 128, NOWN])
    mF = scr("mF", [DC, 128, NOWN], F32); mergedT = scr("mergedT", [DC, 128, NOWN])
    x2 = scr("x2", [NOWN, D], F32); tT = scr("tT", [DC, 128, NOWN]); qpT = scr("qpT", [2 * PH, 128, NOWN])
    G_d = scr("G_d", [NOWN, NE]); UT_d = scr("UT_d", [D, NE]); AT_d = scr("AT_d", [NE // 128, 128, NOWN])
    x3 = scr("x3", [NOWN, D], F32)
    with ExitStack() as ctx:
        G = ctx.enter_context(nc.semaphore("G"))
        ident = ctx.enter_context(nc.sbuf_tensor("identb", [128, 128], BF16))
        p = Prog(nc)
        p.dma_cast(ident[:], ident_in)

        def store_T_epi(sctx, dst, dt, name, func=None):
            stt = sctx.enter_context(nc.sbuf_tensor(name, [128, 512], dt))

            def epi(ps, tok0, ntok, m0, nm):
                if func is None:
                    p.v("tensor_copy", stt[:, 0:ntok], ps)
                else:
                    p.a("activation", stt[:, 0:ntok], ps, func)
                p.dma_deferred(dst[m0 // 128, :, tok0:tok0 + ntok], stt[:, 0:ntok])
            return epi

        with ExitStack() as sctx:
            stage_norm_T(p, nc, sctx, x_own, norm_mix, hT_own, NOWN, D, ident)
        with ExitStack() as sctx:
            stage_norm_T(p, nc, sctx, x_kv, norm_mix, hT_kv, NKV, D, ident)
        with ExitStack() as sctx:
            epq = QKEpi(p, nc, sctx, gq, cos_q, sin_q, qT, ident, "q")
            stage_gemm(p, nc, sctx, hT_own, w_in[:, 0:AW], NOWN, D, AW, "T", epq, "q")
        with ExitStack() as sctx:
            epk = QKEpi(p, nc, sctx, gk, cos_k, sin_k, kT, ident, "k")
            stage_gemm(p, nc, sctx, hT_kv, w_in[:, AW:AW + KW], NKV, D, KW, "T", epk, "k")
        with ExitStack() as sctx:
            st = sctx.enter_context(nc.sbuf_tensor("stv", [128, 512], BF16))

            def epv(ps, tok0, ntok, m0, nm):
                p.v("tensor_copy", st[:, 0:nm], ps)
                p.dma_deferred(vt[tok0:tok0 + ntok, m0:m0 + nm], st[:, 0:nm])
            stage_gemm(p, nc, sctx, hT_kv, w_in[:, AW + KW:AW + 2 * KW], NKV, D, KW, "T", epv, "v")
        with ExitStack() as sctx:
            stage_attention(p, nc, sctx, qT, kT, vt, attnT, gq, gk, cfg["segs"], NH, NKVH, "a")
        if LH > SEGL:
            with ExitStack() as sctx:
                stage_gemm(p, nc, sctx, hT_kv[:, :, 0:LH], w_in[:, cU:cU + SW], LH, D, SW, "F",
                           store_T_epi(sctx, uT_hist, BF16, "st_uh"), "uh")
        with ExitStack() as sctx:
            stage_gemm(p, nc, sctx, hT_own, w_in[:, cU:cU + SW], NOWN, D, SW, "F",
                       store_T_epi(sctx, uT_own, BF16, "st_uo"), "uo")
        with ExitStack() as sctx:
            stage_ssm(p, nc, sctx, uT_hist, uT_own, zT, y_scr, prm, oh_src, LH, SEGL,
                      [(0, True), (SEGL, False)], NP, NJ)
        with ExitStack() as sctx:
            sg = sctx.enter_context(nc.sbuf_tensor("glu_s", [128, 512], F32))
            zt_ = sctx.enter_context(nc.sbuf_tensor("glu_z", [128, 512], BF16))
            so_ = sctx.enter_context(nc.sbuf_tensor("glu_o", [128, 512], BF16))

            def pre_glu(tok0, ntok, m0, nm):
                p.dma(zt_[:, 0:ntok], zT[m0 // 128, :, tok0:tok0 + ntok])

            def epi_glu(ps, tok0, ntok, m0, nm):
                p.a("activation", sg[:, 0:ntok], ps, ACT.Sigmoid)
                p.v("tensor_tensor", so_[:, 0:ntok], zt_[:, 0:ntok], sg[:, 0:ntok], ALU.mult)
                p.dma_deferred(soT[m0 // 128, :, tok0:tok0 + ntok], so_[:, 0:ntok])
            stage_gemm(p, nc, sctx, zT, w_glu, NOWN, SW, SW, "F", epi_glu, "glu", pre=pre_glu)
        with ExitStack() as sctx:
            stage_gemm(p, nc, sctx, hT_own, w_in[:, cG:cG + 2 * D], NOWN, D, 2 * D, "F",
                       store_T_epi(sctx, sgT, BF16, "st_sg", ACT.Sigmoid), "sg")
        with ExitStack() as sctx:
            ga = sctx.enter_context(nc.sbuf_tensor("ua_g", [128, 512], BF16))
            mo = sctx.enter_context(nc.sbuf_tensor("ua_m", [128, 512], F32))

            def pre_ua(tok0, ntok, m0, nm):
                p.dma(ga[:, 0:ntok], sgT[m0 // 128, :, tok0:tok0 + ntok])

            def epi_ua(ps, tok0, ntok, m0, nm):
                p.v("tensor_tensor", mo[:, 0:ntok], ps, ga[:, 0:ntok], ALU.mult)
                p.dma_deferred(mF[m0 // 128, :, tok0:tok0 + ntok], mo[:, 0:ntok])
            stage_gemm(p, nc, sctx, attnT, w_up_attn, NOWN, AW, D, "F", epi_ua, "ua", pre=pre_ua)
        with ExitStack() as sctx:
            gs = sctx.enter_context(nc.sbuf_tensor("us_g", [128, 512], BF16))
            mp = sctx.enter_context(nc.sbuf_tensor("us_p", [128, 512], F32))
            mo2 = sctx.enter_context(nc.sbuf_tensor("us_m", [128, 512], F32))
            mb_ = sctx.enter_context(nc.sbuf_tensor("us_b", [128, 512], BF16))

            def pre_us(tok0, ntok, m0, nm):
                p.dma(gs[:, 0:ntok], sgT[DC + m0 // 128, :, tok0:tok0 + ntok])
                p.dma(mp[:, 0:ntok], mF[m0 // 128, :, tok0:tok0 + ntok])

            def epi_us(ps, tok0, ntok, m0, nm):
                p.v("tensor_tensor", mo2[:, 0:ntok], ps, gs[:, 0:ntok], ALU.mult)
                p.v("tensor_tensor", mb_[:, 0:ntok], mo2[:, 0:ntok], mp[:, 0:ntok], ALU.add)
                p.dma_deferred(mergedT[m0 // 128, :, tok0:tok0 + ntok], mb_[:, 0:ntok])
            stage_gemm(p, nc, sctx, soT, w_up_ssm, NOWN, SW, D, "F", epi_us, "us", pre=pre_us)
        with ExitStack() as sctx:
            xr = sctx.enter_context(nc.sbuf_tensor("wo_x", [128, 512], F32))
            xo = sctx.enter_context(nc.sbuf_tensor("wo_o", [128, 512], F32))

            def pre_wo(tok0, ntok, m0, nm):
                p.dma(xr[:, 0:nm], x_own[tok0:tok0 + ntok, m0:m0 + nm])

            def epi_wo(ps, tok0, ntok, m0, nm):
                p.v("tensor_tensor", xo[:, 0:nm], ps, xr[:, 0:nm], ALU.add)
                p.dma_deferred(x2[tok0:tok0 + ntok, m0:m0 + nm], xo[:, 0:nm])
            stage_gemm(p, nc, sctx, mergedT, w_out, NOWN, D, D, "T", epi_wo, "wo", pre=pre_wo)
        with ExitStack() as sctx:
            stage_norm_T(p, nc, sctx, x2, norm_ffn, tT, NOWN, D, ident)
        with ExitStack() as sctx:
            stage_peer_ut(p, nc, sctx, U_src, UT_d, NE, D, ident)
        with ExitStack() as sctx:
            stage_gemm(p, nc, sctx, tT, w_query, NOWN, D, PH * 256, "F",
                       store_T_epi(sctx, qpT, BF16, "st_qp"), "qp")
        with ExitStack() as sctx:
            stage_peer_gate(p, nc, sctx, qpT, skT_src, G_d, NOWN, PH)
        with ExitStack() as sctx:
            ge = sctx.enter_context(nc.sbuf_tensor("pa_ge", [128, 512], F32))
            gl = sctx.enter_context(nc.sbuf_tensor("pa_gl", [128, 512], BF16))
            ab_ = sctx.enter_context(nc.sbuf_tensor("pa_a", [128, 512], BF16))
            at_ = sctx.enter_context(nc.sbuf_tensor("pa_at", [128, 4, 128], BF16))
            pta = sctx.enter_context(nc.psum_tensor("pa_pt", [128, 4, 128], BF16))

            def pre_pa(tok0, ntok, m0, nm):
                p.dma(gl[:, 0:nm], G_d[tok0:tok0 + ntok, m0:m0 + nm])

            def epi_pa(ps, tok0, ntok, m0, nm):
                p.a("activation", ge[:, 0:nm], ps, ACT.Gelu_apprx_tanh)
                p.v("tensor_tensor", ab_[:, 0:nm], ge[:, 0:nm], gl[:, 0:nm], ALU.mult)
                nb = nm // 128
                with p.par():
                    for c in range(nb):
                        p.pe("transpose", pta[:, c, :], ab_[:, c * 128:(c + 1) * 128], ident[:])
                p.v("tensor_copy", at_[:, 0:nb, :], pta[:, 0:nb, :])
                p.dma_deferred(AT_d[m0 // 128:m0 // 128 + nb, :, tok0:tok0 + ntok].rearrange("c e t -> e c t"), at_[:, 0:nb, :])
            stage_gemm(p, nc, sctx, tT, UT_d, NOWN, D, NE, "T", epi_pa, "pa", w_bf16=True, pre=pre_pa)
        with ExitStack() as sctx:
            xr2 = sctx.enter_context(nc.sbuf_tensor("pb_x", [128, 8, 512], F32))
            xo2 = sctx.enter_context(nc.sbuf_tensor("pb_o", [128, 512], F32))

            def pre_pb(t, tok0, ntok, m0, nm):
                p.dma(xr2[:, t, 0:nm], x2[tok0:tok0 + ntok, m0:m0 + nm])

            def epi_pb(ps, tok0, ntok, m0, nm, t):
                p.v("tensor_tensor", xo2[:, 0:nm], ps, xr2[:, t, 0:nm], ALU.add)
                p.dma_deferred(x3[tok0:tok0 + ntok, m0:m0 + nm], xo2[:, 0:nm])
            stage_gemm_bigk(p, nc, sctx, AT_d, V_src, NOWN, NE, D, epi_pb, "pb", pre=pre_pb)
        with ExitStack() as sctx:
            stage_norm_tok(p, nc, sctx, x3, norm_final, y_out, NOWN, D)
        p.finish(G)
    return nc


FULL = dict(D=4096, NOWN=4096, SEGL=2048, LH=16384, NH=16, NKVH=4, SW=2048, PH=8, NE=16384,
            segs=[(0, 2048, 0, 16384), (2048, 2048, 16384, 2048)])


def _rope_tables(length):
    rows = length // 64
    row = np.repeat(np.arange(rows), 64).astype(np.float32)
    col = np.tile(np.arange(64), rows).astype(np.float32)
    inv = (10000.0 ** (-np.arange(0, 64, 2, dtype=np.float32) / 64)).astype(np.float32)
    ang = np.concatenate([row[:, None] * inv, col[:, None] * inv], axis=-1).astype(np.float32)
    return np.cos(ang).astype(np.float32), np.sin(ang).astype(np.float32)


def make_in_maps(inputs, cfg, n_cores):
    g = lambda k: np.asarray(inputs[k])
    SEGL, LH = cfg["SEGL"], cfg["LH"]
    xp = g("x_prompt")[0]
    xs = g("x_sample")
    cp, sp = _rope_tables(LH)
    cs, ss = _rope_tables(SEGL)
    cos_k = np.concatenate([cp, cs], 0); sin_k = np.concatenate([sp, ss], 0)
    shared = {
        "norm_mix": np.ascontiguousarray(g("norm_mix")[0]), "norm_ffn": np.ascontiguousarray(g("norm_ffn")[0]),
        "norm_final": np.ascontiguousarray(g("norm_final")),
        "w_in": np.ascontiguousarray(g("w_in")[0]), "q_norm": np.ascontiguousarray(g("q_norm")[0]),
        "k_norm": np.ascontiguousarray(g("k_norm")[0]), "cos_k": cos_k, "sin_k": sin_k,
        "ident": np.eye(128, dtype=np.float32),
        "w_glu": np.ascontiguousarray(g("w_glu")[0]), "w_up_attn": np.ascontiguousarray(g("w_up_attn")[0]),
        "w_up_ssm": np.ascontiguousarray(g("w_up_ssm")[0]), "w_out": np.ascontiguousarray(g("w_out")[0]),
        "peer_w_query": np.ascontiguousarray(g("peer_w_query")[0]),
        "peer_skT": np.ascontiguousarray(g("peer_sub_keys")[0].reshape(-1, 128, 128).transpose(2, 0, 1)),
        "peer_u": np.ascontiguousarray(g("peer_u")[0]), "peer_v": np.ascontiguousarray(g("peer_v")[0]),
    }
    shared.update(ssm_host_layout(g("ssm_A_re")[0], g("ssm_A_im")[0], g("ssm_log_dt")[0], g("ssm_B_re")[0],
                                  g("ssm_B_im")[0], g("ssm_C_re")[0], g("ssm_C_im")[0], g("ssm_D")[0]))
    in_maps = []
    for i in range(n_cores):
        sl = slice(SEGL * i, SEGL * (i + 1))
        m = dict(shared)
        m["x_own"] = np.ascontiguousarray(np.concatenate([xp[sl], xs[i]], axis=0))
        m["x_kv"] = np.ascontiguousarray(np.concatenate([xp, xs[i]], axis=0))
        m["cos_q"] = np.ascontiguousarray(np.concatenate([cp[sl], cs], 0))
        m["sin_q"] = np.ascontiguousarray(np.concatenate([sp[sl], ss], 0))
        oh = np.zeros(8, np.float32); oh[i] = 1.0
        m["onehot"] = oh
        in_maps.append(m)
    return in_maps


def kernel(**inputs):
    cfg = FULL
    nc = build_program(cfg)
    in_maps = make_in_maps(inputs, cfg, 8)
    res = run_bass_kernel_spmd(nc, in_maps, core_ids=list(range(8)))
    S = cfg["SEGL"]
    yp = np.concatenate([r["y_out"][:S] for r in res.results], axis=0)[None]
    ys = np.stack([r["y_out"][S:] for r in res.results], axis=0)
    return (yp.astype(np.float32), ys.astype(np.float32))
```

```python
import numpy as np
import ml_dtypes
import concourse.bass as bass
import concourse.mybir as mybir
from concourse.bass_utils import run_bass_kernel_spmd

F32 = mybir.dt.float32
BF16 = mybir.dt.bfloat16
ALU = mybir.AluOpType
ACT = mybir.ActivationFunctionType
AX = mybir.AxisListType

EPS = 1e-6


class Prog:
    ENGS = ("sync", "scalar", "gpsimd", "tensor", "vector")

    def __init__(self, nc):
        self.nc = nc
        self.q = {e: [] for e in self.ENGS}
        self.cum = 0
        self.barrier = 0
        self.in_par = False
        self.deferred = []

    def emit(self, eng, fn, dma=False):
        inc = 16 if dma else 1
        self.q[eng].append((fn, self.barrier, inc))
        self.cum += inc
        if not self.in_par:
            self.barrier = self.cum

    class _Par:
        def __init__(self, p):
            self.p = p

        def __enter__(self):
            self.p.in_par = True

        def __exit__(self, *a):
            self.p.in_par = False
            self.p.barrier = self.p.cum

    def par(self):
        return Prog._Par(self)

    def dma(self, out, in_, eng="sync", **kw):
        self.emit(eng, lambda e: e.dma_start(out=out, in_=in_, **kw), dma=True)

    def dma_cast(self, out, in_, **kw):
        self.emit("gpsimd", lambda e: e.dma_start(out=out, in_=in_, **kw), dma=True)

    def op(self, eng, name, *args, **kw):
        self.emit(eng, lambda e: getattr(e, name)(*args, **kw))

    def v(self, name, *args, **kw):
        self.op("vector", name, *args, **kw)

    def a(self, name, *args, **kw):
        self.op("scalar", name, *args, **kw)

    def g(self, name, *args, **kw):
        self.op("gpsimd", name, *args, **kw)

    def pe(self, name, *args, **kw):
        self.op("tensor", name, *args, **kw)

    def dma_deferred(self, out, in_):
        self.deferred.append((out, in_))

    def flush_deferred(self):
        for out, in_ in self.deferred:
            self.dma(out, in_)
        self.deferred = []

    def finish(self, G):
        self.flush_deferred()
        nc = self.nc
        total = self.cum
        with nc.Block() as block:
            def run(engname):
                def body(e):
                    last = -1
                    for fn, w, inc in self.q[engname]:
                        if w > last and w > 0:
                            e.wait_ge(G, w)
                            last = w
                        fn(e).then_inc(G, inc)
                    e.wait_ge(G, total)
                return body
            block.sync(run("sync"))
            block.scalar(run("scalar"))
            block.gpsimd(run("gpsimd"))
            block.tensor(run("tensor"))
            block.vector(run("vector"))


def stage_norm_T(p, nc, ctx, x_src, g_src, hT_dst, N, D, ident):
    KC = D // 128
    TB = next(t for t in (512, 256, 128) if N % t == 0)
    gb = ctx.enter_context(nc.sbuf_tensor(nc.make_name("gb", True), [128, D], F32))
    xts = [ctx.enter_context(nc.sbuf_tensor(nc.make_name("xt", True), [128, D], F32)) for _ in range(2)]
    xn = ctx.enter_context(nc.sbuf_tensor(nc.make_name("xn", True), [128, D], BF16))
    ss = ctx.enter_context(nc.sbuf_tensor(nc.make_name("ss", True), [128, 2], F32))
    hb = ctx.enter_context(nc.sbuf_tensor(nc.make_name("hb", True), [128, KC, TB], BF16))
    pt = ctx.enter_context(nc.psum_tensor(nc.make_name("ptn", True), [128, 8, 128], BF16))
    with p.par():
        p.dma(gb[:], g_src.partition_broadcast(128))
        p.dma(xts[0][:], x_src[0:128, :])
    ntile = N // 128
    for k in range(ntile):
        t0 = k * 128
        b0 = (t0 // TB) * TB
        ti = (t0 - b0) // 128
        xt = xts[k % 2]
        p.a("activation", xn[:], xt[:], ACT.Square, accum_out=ss[:, 0:1])
        p.a("activation", ss[:, 1:2], ss[:, 0:1], ACT.Sqrt, bias=EPS, scale=1.0 / D)
        p.v("reciprocal", ss[:, 1:2], ss[:, 1:2])
        p.v("scalar_tensor_tensor", xn[:], xt[:], ss[:, 1:2], gb[:], ALU.mult, ALU.mult)
        for c0 in range(0, KC, 8):
            nb = min(8, KC - c0)
            with p.par():
                if k + 1 < ntile:
                    p.dma(xts[(k + 1) % 2][:, c0 * 128:(c0 + nb) * 128], x_src[t0 + 128:t0 + 256, c0 * 128:(c0 + nb) * 128])
                for c in range(nb):
                    p.pe("transpose", pt[:, c, :], xn[:, (c0 + c) * 128:(c0 + c + 1) * 128], ident[:])
            p.v("tensor_copy", hb[:, c0:c0 + nb, ti * 128:(ti + 1) * 128], pt[:, 0:nb, :])
        if ti == TB // 128 - 1:
            p.dma(hT_dst[:, :, b0:b0 + TB].rearrange("k d n -> d k n"), hb[:])


def stage_gemm(p, nc, ctx, actT, w_src, N, K, M, orient, epi, tag, w_bf16=False, pre=None):
    KC = K // 128
    TB = min(1024, N)
    ab = ctx.enter_context(nc.sbuf_tensor(nc.make_name("ab" + tag, True), [128, KC, TB], BF16))
    wts = [ctx.enter_context(nc.sbuf_tensor(nc.make_name("wt%d" % i + tag, True), [128, KC, 512], BF16)) for i in range(2)]
    ps = ctx.enter_context(nc.psum_tensor(nc.make_name("ps" + tag, True), [128, 512], F32))
    units = [(tb, mb) for tb in range(0, N, TB) for mb in range(0, M, 512)]

    def wload(u, k0, k1):
        tb, mb = units[u]
        mw = min(512, M - mb)
        wsrc_ap = w_src[k0 * 128:k1 * 128, mb:mb + mw].rearrange("(k d) m -> d k m", d=128)
        (p.dma if w_bf16 else p.dma_cast)(wts[u % 2][:, k0:k1, 0:mw], wsrc_ap)

    with p.par():
        for k0 in range(0, KC, 8):
            wload(0, k0, min(KC, k0 + 8))
    for u, (tb, mb) in enumerate(units):
        wt = wts[u % 2]
        mw = min(512, M - mb)
        if mb == 0:
            with p.par():
                for k0 in range(0, KC, 8):
                    k1 = min(KC, k0 + 8)
                    p.dma(ab[:, k0:k1, :], actT[k0:k1, :, tb:tb + TB].rearrange("k d n -> d k n"))
        if orient == "T":
            groups = [("T", tt) for tt in range(0, TB, 128)]
        else:
            groups = [("F", ms, ts) for ms in range(0, mw, 128) for ts in range(0, TB, 512)]
        ng = len(groups)
        cuts = [(KC * gi) // ng for gi in range(ng + 1)]
        for gi, g in enumerate(groups):
            with p.par():
                p.flush_deferred()
                if pre is not None:
                    if g[0] == "T":
                        pre(tb + g[1], 128, mb, mw)
                    else:
                        pre(tb + g[2], min(512, TB - g[2]), mb + g[1], 128)
                if u + 1 < len(units) and cuts[gi + 1] > cuts[gi]:
                    wload(u + 1, cuts[gi], cuts[gi + 1])
                if g[0] == "T":
                    tt = g[1]
                    for kc in range(KC):
                        p.pe("matmul", ps[:, 0:mw], ab[:, kc, tt:tt + 128], wt[:, kc, 0:mw],
                             start=(kc == 0), stop=(kc == KC - 1))
                else:
                    ms, ts = g[1], g[2]
                    tw = min(512, TB - ts)
                    for kc in range(KC):
                        p.pe("matmul", ps[:, 0:tw], wt[:, kc, ms:ms + 128], ab[:, kc, ts:ts + tw],
                             start=(kc == 0), stop=(kc == KC - 1))
            if g[0] == "T":
                epi(ps[:, 0:mw], tb + g[1], 128, mb, mw)
            else:
                tw = min(512, TB - g[2])
                epi(ps[:, 0:tw], tb + g[2], tw, mb + g[1], 128)
    p.flush_deferred()


def apv(t, off, dims):
    ps = 1
    for d in t.shape[1:]:
        ps *= d
    return bass.AP(t, off, [[ps, 128]] + [list(d) for d in dims])


class QKEpi:
    def __init__(self, p, nc, ctx, gvec_src, cos_src, sin_src, dstT, ident, tag):
        self.p, self.nc, self.dstT, self.ident = p, nc, dstT, ident
        self.cos_src, self.sin_src = cos_src, sin_src
        mk = lambda n, sh, dt: ctx.enter_context(nc.sbuf_tensor(nc.make_name(n + tag, True), sh, dt))
        self.t32 = mk("qe_t", [128, 512], F32)
        self.sq = mk("qe_s", [128, 512], F32)
        self.sq2 = mk("qe_s2", [128, 512], F32)
        self.ssh = mk("qe_h", [128, 8], F32)
        self.gqb = mk("qe_g", [128, 128], F32)
        self.cs = mk("qe_c", [128, 2, 64], F32)
        self.rb = mk("qe_r", [128, 512], BF16)
        self.qTs = mk("qe_q", [128, 4, 128], BF16)
        self.ptq = ctx.enter_context(nc.psum_tensor(nc.make_name("qe_p" + tag, True), [128, 4, 128], BF16))
        p.dma(self.gqb[:], gvec_src.partition_broadcast(128))
        self.cur_tok = None

    def __call__(self, ps, tok0, ntok, m0, nm):
        p = self.p
        nh = nm // 128
        t32, sq, sq2, ssh, rb = self.t32, self.sq, self.sq2, self.ssh, self.rb
        if self.cur_tok != tok0:
            with p.par():
                p.dma(self.cs[:, 0, :], self.cos_src[tok0:tok0 + 128, :])
                p.dma(self.cs[:, 1, :], self.sin_src[tok0:tok0 + 128, :])
            self.cur_tok = tok0
        p.v("tensor_copy", t32[:, 0:nm], ps)
        p.v("tensor_tensor", sq[:, 0:nm], t32[:, 0:nm], t32[:, 0:nm], ALU.mult)
        p.v("tensor_reduce", ssh[:, 0:nh], apv(sq, 0, [[128, nh], [1, 128]]), AX.X, ALU.add)
        p.a("activation", ssh[:, 4:4 + nh], ssh[:, 0:nh], ACT.Sqrt, bias=EPS, scale=1.0 / 128)
        p.v("reciprocal", ssh[:, 4:4 + nh], ssh[:, 4:4 + nh])
        v3 = lambda t: apv(t, 0, [[128, nh], [1, 128]])
        p.v("tensor_tensor", v3(t32), v3(t32), apv(ssh, 4, [[1, nh], [0, 128]]), ALU.mult)
        p.v("tensor_tensor", v3(t32), v3(t32), apv(self.gqb, 0, [[0, nh], [1, 128]]), ALU.mult)
        x = lambda t, half: apv(t, half * 32, [[128, nh], [64, 2], [1, 32]])
        cst = lambda which: apv(self.cs, which * 64, [[0, nh], [32, 2], [1, 32]])
        p.v("tensor_tensor", x(sq, 0), x(t32, 0), cst(0), ALU.mult)
        p.v("tensor_tensor", x(sq, 1), x(t32, 1), cst(1), ALU.mult)
        p.v("tensor_tensor", x(rb, 0), x(sq, 0), x(sq, 1), ALU.subtract)
        p.v("tensor_tensor", x(sq2, 0), x(t32, 0), cst(1), ALU.mult)
        p.v("tensor_tensor", x(sq2, 1), x(t32, 1), cst(0), ALU.mult)
        p.v("tensor_tensor", x(rb, 1), x(sq2, 0), x(sq2, 1), ALU.add)
        with p.par():
            for h in range(nh):
                p.pe("transpose", self.ptq[:, h, :], rb[:, h * 128:(h + 1) * 128], self.ident[:])
        p.v("tensor_copy", self.qTs[:, 0:nh, :], self.ptq[:, 0:nh, :])
        h0 = m0 // 128
        p.dma_deferred(self.dstT[h0:h0 + nh, :, tok0:tok0 + 128].rearrange("h d n -> d h n"), self.qTs[:, 0:nh, :])


def stage_attention(p, nc, ctx, qT_src, kT_src, v_src, attnT_dst, gq_src, gk_src, segs, NH, NKV, tag):
    GQ = NH // NKV
    NKmax = max(s[3] for s in segs)
    NQmax = max(s[1] for s in segs)
    mk = lambda n, sh, dt: ctx.enter_context(nc.sbuf_tensor(nc.make_name(n + tag, True), sh, dt))
    kT = mk("at_k", [128, NKmax], BF16)
    vv = mk("at_v", [128, NKmax // 128, 128], BF16)
    qTt = mk("at_q", [128, NQmax], BF16)
    pT = mk("at_p", [128, 4, 512], BF16)
    ones = mk("at_1", [128, 128], BF16)
    rc = mk("at_rc", [128, 512], F32)
    obb = mk("at_ob", [128, 512], BF16)
    gg = mk("at_g", [128, 2, 128], F32)
    cb = mk("at_cb", [128, 4], F32)
    sc = ctx.enter_context(nc.psum_tensor(nc.make_name("at_sc" + tag, True), [128, 4, 512], F32))
    acc_o = ctx.enter_context(nc.psum_tensor(nc.make_name("at_ao" + tag, True), [128, 512], F32))
    acc_s = ctx.enter_context(nc.psum_tensor(nc.make_name("at_as" + tag, True), [128, 512], F32))
    scale = 128 ** -0.5
    p.v("memset", ones[:], 1.0)
    with p.par():
        p.dma(gg[:, 0, :], gq_src.partition_broadcast(128))
        p.dma(gg[:, 1, :], gk_src.partition_broadcast(128))
    p.v("tensor_reduce", cb[:, 0:2], gg[:], AX.X, ALU.max, apply_absolute_value=True)
    p.v("tensor_tensor", cb[:, 2:3], cb[:, 0:1], cb[:, 1:2], ALU.mult)
    p.v("tensor_scalar", cb[:, 3:4], cb[:, 2:3], -(128 ** 0.5), None, ALU.mult)
    for (qoff, NQ, koff, NK) in segs:
        NKC = NK // 128
        NG = NKC // 2
        for kvh in range(NKV):
            with p.par():
                p.dma(kT[:, 0:NK], kT_src[kvh, :, koff:koff + NK])
                p.dma(vv[:, 0:NKC, :], v_src[koff:koff + NK, kvh * 128:(kvh + 1) * 128].rearrange("(c p) d -> p c d", p=128))
            for qh in range(GQ):
                head = kvh * GQ + qh
                p.dma(qTt[:, 0:NQ], qT_src[head, :, qoff:qoff + NQ])
                for qb in range(0, NQ, 512):
                    qw = min(512, NQ - qb)
                    for n in range(-1, NG + 1):
                        with p.par():
                            if 0 <= n - 1:
                                m = n - 1
                                for j in range(2):
                                    kc = 2 * m + j
                                    first = (kc == 0); last = (kc == NKC - 1)
                                    p.pe("matmul", acc_o[:, 0:qw], vv[:, kc, :], pT[:, 2 * (m % 2) + j, 0:qw], start=first, stop=last)
                                    p.pe("matmul", acc_s[:, 0:qw], ones[:], pT[:, 2 * (m % 2) + j, 0:qw], start=first, stop=last)
                            if n + 1 < NG:
                                m = n + 1
                                for j in range(2):
                                    kc = 2 * m + j
                                    p.pe("matmul", sc[:, 2 * (m % 2) + j, 0:qw], kT[:, kc * 128:(kc + 1) * 128],
                                         qTt[:, qb:qb + qw], start=True, stop=True)
                            if 0 <= n < NG:
                                b = 2 * (n % 2)
                                p.a("activation", pT[:, b:b + 2, 0:qw], sc[:, b:b + 2, 0:qw], ACT.Exp, bias=cb[:, 3:4], scale=scale)
                    p.v("reciprocal", rc[:, 0:qw], acc_s[:, 0:qw])
                    p.v("tensor_tensor", obb[:, 0:qw], acc_o[:, 0:qw], rc[:, 0:qw], ALU.mult)
                    p.dma(attnT_dst[head, :, qoff + qb:qoff + qb + qw], obb[:, 0:qw])


import math


def ssm_host_layout(A_re, A_im, log_dt, B_re, B_im, C_re, C_im, D):
    _, H, P = A_re.shape
    Gs = B_re.shape[-1]
    NP = H // 2
    NJ = H * Gs // 128
    lay = lambda a: np.ascontiguousarray(a.reshape(2, NP, 2 * P).transpose(2, 0, 1))
    dte = np.ascontiguousarray(np.repeat(log_dt.reshape(2, NP, 2, 1), P, axis=3).reshape(2, NP, 2 * P).transpose(2, 0, 1))
    Bt = np.zeros((128, 2, 2, NJ, 128), np.float32)
    Bt3 = np.zeros((128, 2, 2, NJ, 128), np.float32)
    Ct = np.zeros((128, 2, 2, NP, 64), np.float32)
    for ri, (Bx, Cx) in enumerate(((B_re, C_re), (B_im, C_im))):
        for pair in range(NP):
            j, q = pair // 4, pair % 4
            for h2 in range(2):
                h = 2 * pair + h2
                blk = Bx[:, h].transpose(2, 0, 1)
                r0 = 32 * q + 16 * h2
                (Bt if q % 2 == 0 else Bt3)[r0:r0 + Gs, :, ri, j, h2 * P:(h2 + 1) * P] = blk
                c0 = (q % 2) * 32 + 16 * h2
                Ct[h2 * P:(h2 + 1) * P, :, ri, pair, c0:c0 + Gs] = Cx[:, h].transpose(2, 0, 1)
    Dt = np.ascontiguousarray(D.reshape(NJ, 128).T)
    return dict(ssm_Ar=lay(A_re), ssm_Ai=lay(A_im), ssm_dt=dte, ssm_Bt=Bt, ssm_Bt3=Bt3, ssm_Ct=Ct, ssm_Dt=Dt)


def stage_ssm(p, nc, ctx, uT_hist, uT_own, zT_own, y_scr, prm, oh_src, LH, SEGL, seqs, NP, NJ, tag=""):
    from contextlib import ExitStack
    TBK = 64
    NSEG = LH // SEGL
    PPU = min(32, NP)
    mk = lambda n, sh, dt, c=ctx: c.enter_context(nc.sbuf_tensor(nc.make_name("ss_" + n + tag, True), sh, dt))
    A2 = mk("A2", [128, 2, 2, NP], F32); B2 = mk("B2", [128, 2, 2, NP], F32)
    Bt = mk("Bt", [128, 2, 2, NJ, 128], BF16); Bt3 = mk("Bt3", [128, 2, 2, NJ, 128], BF16)
    Cb = mk("Cb", [128, 2, 2, NP, 64], BF16)
    Dt = mk("Dt", [128, NJ], F32)
    oh = mk("oh", [128, 8], F32)
    Xc = mk("Xc", [128, 2, 2, NP], F32); Xin = mk("Xin", [128, 2, 2, NP], F32)
    m1 = mk("m1", [128, 2, 2, NP], F32); m2 = mk("m2", [128, 2, 2, NP], F32)
    with ExitStack() as c2:
        mk2 = lambda n, sh, dt: mk(n, sh, dt, c2)
        Ar = mk2("ar", [128, 2, NP], F32); Ai = mk2("ai", [128, 2, NP], F32); dtv = mk2("dt", [128, 2, NP], F32)
        t1 = mk2("t1", [128, 2, NP], F32); t2 = mk2("t2", [128, 2, NP], F32); t3 = mk2("t3", [128, 2, NP], F32)
        cc = mk2("cc", [128, 2, NP], F32); sn = mk2("sn", [128, 2, NP], F32)
        lbr = mk2("lbr", [128, 2, NP], F32); lbi = mk2("lbi", [128, 2, NP], F32)
        cr = mk2("cr", [128, 2, NP], F32); ci = mk2("ci", [128, 2, NP], F32)
        Ctf = mk2("Ctf", [128, 2, NP, 64], F32)
        W1 = mk2("W1", [128, NP, 64], F32); W2 = mk2("W2", [128, NP, 64], F32)
        with p.par():
            p.dma(Ar[:], prm["Ar"]); p.dma(Ai[:], prm["Ai"]); p.dma(dtv[:], prm["dt"])
            p.dma(Dt[:], prm["Dt"]); p.dma(oh[:], oh_src.partition_broadcast(128))
        for d in range(2):
            p.dma_cast(Bt[:, d], prm["Bt"][:, d])
            p.dma_cast(Bt3[:, d], prm["Bt3"][:, d])
        p.a("activation", dtv[:], dtv[:], ACT.Exp)
        p.v("tensor_tensor", t1[:], Ar[:], dtv[:], ALU.mult)
        p.v("tensor_tensor", t2[:], Ai[:], dtv[:], ALU.mult)
        p.a("activation", lbr[:], t1[:], ACT.Exp)
        p.a("activation", sn[:], t2[:], ACT.Sin, scale=1.0 / 32)
        p.v("tensor_scalar", t3[:], t2[:], 1.0 / 32, math.pi / 2, ALU.mult, ALU.add)
        p.a("activation", cc[:], t3[:], ACT.Sin)
        for _ in range(5):
            p.v("tensor_tensor", t1[:], cc[:], cc[:], ALU.mult)
            p.v("tensor_tensor", t2[:], sn[:], sn[:], ALU.mult)
            p.v("tensor_tensor", t3[:], cc[:], sn[:], ALU.mult)
            p.v("tensor_tensor", cc[:], t1[:], t2[:], ALU.subtract)
            p.v("tensor_scalar", sn[:], t3[:], 2.0, None, ALU.mult)
        p.v("tensor_tensor", lbi[:], lbr[:], sn[:], ALU.mult)
        p.v("tensor_tensor", lbr[:], lbr[:], cc[:], ALU.mult)
        for ri in range(2):
            p.v("tensor_copy", A2[:, :, ri, :], lbr[:])
        p.v("tensor_scalar", B2[:, :, 0, :], lbi[:], -1.0, None, ALU.mult)
        p.v("tensor_copy", B2[:, :, 1, :], lbi[:])
        p.v("tensor_scalar", t1[:], lbr[:], -1.0, None, ALU.add)
        p.v("tensor_tensor", t2[:], Ar[:], Ar[:], ALU.mult)
        p.v("tensor_tensor", t3[:], Ai[:], Ai[:], ALU.mult)
        p.v("tensor_tensor", t2[:], t2[:], t3[:], ALU.add)
        p.v("reciprocal", t2[:], t2[:])
        p.v("tensor_tensor", cr[:], t1[:], Ar[:], ALU.mult)
        p.v("tensor_tensor", t3[:], lbi[:], Ai[:], ALU.mult)
        p.v("tensor_tensor", cr[:], cr[:], t3[:], ALU.add)
        p.v("tensor_tensor", cr[:], cr[:], t2[:], ALU.mult)
        p.v("tensor_tensor", ci[:], lbi[:], Ar[:], ALU.mult)
        p.v("tensor_tensor", t3[:], t1[:], Ai[:], ALU.mult)
        p.v("tensor_tensor", ci[:], ci[:], t3[:], ALU.subtract)
        p.v("tensor_tensor", ci[:], ci[:], t2[:], ALU.mult)
        for d in range(2):
            p.dma(Ctf[:], prm["Ct"][:, d])
            bc = lambda t: apv(t, d * NP, [[1, NP], [0, 64]])
            p.v("tensor_tensor", W1[:], Ctf[:, 0], bc(cr), ALU.mult)
            p.v("tensor_tensor", W2[:], Ctf[:, 1], bc(ci), ALU.mult)
            p.v("tensor_tensor", Cb[:, d, 0], W1[:], W2[:], ALU.subtract)
            p.v("tensor_tensor", W1[:], Ctf[:, 0], bc(ci), ALU.mult)
            p.v("tensor_tensor", W2[:], Ctf[:, 1], bc(cr), ALU.mult)
            p.v("tensor_tensor", W1[:], W1[:], W2[:], ALU.add)
            p.v("tensor_scalar", Cb[:, d, 1], W1[:], -1.0, None, ALU.mult)
    XB = mk("XB", [128, 2, 2, NP, TBK], F32)
    Xbf = mk("Xbf", [128, 2, NP, TBK], BF16)
    ufs = [mk("uf%d" % i, [128, NJ, TBK], BF16) for i in range(2)]
    ubs = [mk("ub%d" % i, [128, NJ, TBK], BF16) for i in range(2)]
    NB = SEGL // TBK
    hist_steps = ([(uT_hist, TBK * n, LH - TBK * (n + 1)) for n in range(LH // TBK) if TBK * n < LH - SEGL]
                  if NSEG > 1 else [])
    own_steps = [(uT_own, o + TBK * n, o + SEGL - TBK * (n + 1)) for (o, _) in seqs for n in range(NB)]
    steps = hist_steps + own_steps

    def load_u(i):
        usrc, a_, b_ = steps[i]
        p.dma(ufs[i % 2][:], usrc[:, :, a_:a_ + TBK].rearrange("j c t -> c j t"))
        p.dma(ubs[i % 2][:], usrc[:, :, b_:b_ + TBK].rearrange("j c t -> c j t"))
    yt = mk("yt", [128, NJ, TBK], F32); yp = mk("yp", [128, NJ, TBK], F32); zb = mk("zb", [128, NJ, TBK], BF16)
    psu = ctx.enter_context(nc.psum_tensor(nc.make_name("ss_psu" + tag, True), [128, PPU, TBK], F32))
    psy = ctx.enter_context(nc.psum_tensor(nc.make_name("ss_psy" + tag, True), [128, NJ, TBK], F32))
    RS = NP * TBK
    DS = 2 * RS
    PS = 2 * DS

    def slot(sf, sb):
        return bass.AP(XB, sf, [[PS, 128], [DS + sb - sf, 2], [RS, 2], [TBK, NP]])

    def slot_ri(sf, sb, ri):
        return bass.AP(XB, sf + ri * RS, [[PS, 128], [DS + sb - sf, 2], [TBK, NP]])

    def block_step(si):
        uf, ub = ufs[si % 2], ubs[si % 2]
        for d in range(2):
            u = uf if d == 0 else ub
            for ri in range(2):
                for pb in range(0, NP, PPU):
                    jb, nj = pb // 4, PPU // 4
                    for hf in range(2):
                        with p.par():
                            for jj in range(nj):
                                j = jb + jj
                                for q in (2 * hf, 2 * hf + 1):
                                    tb_ = Bt if q % 2 == 0 else Bt3
                                    p.pe("matmul", psu[:, 4 * jj + q, :], tb_[64 * hf:64 * hf + 64, d, ri, j, :],
                                         u[64 * hf:64 * hf + 64, j, :], start=True, stop=True)
                    hp_ = PPU // 2
                    with p.par():
                        p.a("copy", XB[:, d, ri, pb:pb + hp_, :], psu[:, 0:hp_, :])
                        p.v("tensor_copy", XB[:, d, ri, pb + hp_:pb + PPU, :], psu[:, hp_:PPU, :])
        with p.par():
            if si + 1 < len(steps):
                load_u(si + 1)
            for s_ in range(TBK):
                prev = Xc[:] if s_ == 0 else slot(s_ - 1, TBK - s_)
                pr = (lambda r: Xc[:, :, r, :]) if s_ == 0 else (lambda r: slot_ri(s_ - 1, TBK - s_, r))
                cur = slot(s_, TBK - 1 - s_)
                p.v("tensor_tensor", m1[:], prev, A2[:], ALU.mult)
                p.v("tensor_tensor", m2[:, :, 0, :], pr(1), B2[:, :, 0, :], ALU.mult)
                p.v("tensor_tensor", m2[:, :, 1, :], pr(0), B2[:, :, 1, :], ALU.mult)
                p.v("tensor_tensor", m1[:], m1[:], m2[:], ALU.add)
                p.v("tensor_tensor", cur, m1[:], cur, ALU.add)
        p.v("tensor_copy", Xc[:], slot(TBK - 1, 0))

    p.v("memset", Xin[:], 0.0)
    p.v("memset", Xc[:], 0.0)
    with p.par():
        load_u(0)
    si = 0
    if NSEG > 1:
        for n in range(len(hist_steps)):
            block_step(si); si += 1
            done = TBK * (n + 1)
            if done % SEGL == 0:
                k = done // SEGL
                p.v("scalar_tensor_tensor", Xin[:, 0], Xc[:, 0], oh[:, k:k + 1], Xin[:, 0], ALU.mult, ALU.add)
                kb = NSEG - 1 - k
                p.v("scalar_tensor_tensor", Xin[:, 1], Xc[:, 1], oh[:, kb:kb + 1], Xin[:, 1], ALU.mult, ALU.add)
    for (o, use_hist) in seqs:
        if use_hist:
            p.v("tensor_copy", Xc[:], Xin[:])
        else:
            p.v("memset", Xc[:], 0.0)
        for n in range(NB):
            tf0 = o + TBK * n
            tb0 = o + SEGL - TBK * (n + 1)
            uf, ub = ufs[si % 2], ubs[si % 2]
            block_step(si); si += 1
            for d in range(2):
                tok0 = tf0 if d == 0 else tb0
                u = uf if d == 0 else ub
                with p.par():
                    p.a("copy", Xbf[:, 0], XB[:, d, 0])
                    p.v("tensor_copy", Xbf[:, 1], XB[:, d, 1])
                for half in range(2):
                    with p.par():
                        for j in range(NJ):
                            for qq in range(2):
                                pair = 4 * j + 2 * half + qq
                                for ri in range(2):
                                    p.pe("matmul", psy[64 * half:64 * half + 64, j, :], Cb[:, d, ri, pair, :],
                                         Xbf[:, ri, pair, :], start=(qq == 0 and ri == 0), stop=(qq == 1 and ri == 1))
                dst = y_scr[:, :, tok0:tok0 + TBK].rearrange("j c t -> c j t")
                if n < NB // 2:
                    p.v("tensor_copy", yt[:], psy[:])
                    p.dma(dst, yt[:])
                else:
                    p.dma(yp[:], dst)
                    p.v("tensor_tensor", yt[:], psy[:], yp[:], ALU.add)
                    p.v("tensor_tensor", yp[:], u[:], apv(Dt, 0, [[1, NJ], [0, TBK]]), ALU.mult)
                    p.v("tensor_tensor", yt[:], yt[:], yp[:], ALU.add)
                    p.a("activation", zb[:], yt[:], ACT.Gelu_apprx_tanh)
                    p.dma(zT_own[:, :, tok0:tok0 + TBK].rearrange("j c t -> c j t"), zb[:])


def stage_gemm_bigk(p, nc, ctx, actT, w_src, N, K, M, epi, tag, pre=None):
    KC = K // 128
    KP = min(16, KC)
    TB = min(1024, N)
    NT = TB // 128
    abs_ = [ctx.enter_context(nc.sbuf_tensor(nc.make_name("bk_a%d" % i + tag, True), [128, KP, TB], BF16)) for i in range(2)]
    wts = [ctx.enter_context(nc.sbuf_tensor(nc.make_name("bk_w%d" % i + tag, True), [128, KP, 512], BF16)) for i in range(2)]
    ps = ctx.enter_context(nc.psum_tensor(nc.make_name("bk_p" + tag, True), [128, NT, 512], F32))
    units = [(tb, mb, k0) for tb in range(0, N, TB) for mb in range(0, M, 512) for k0 in range(0, KC, KP)]

    def loads(u):
        tb, mb, k0 = units[u]
        mw = min(512, M - mb)
        for kk in range(0, KP, 8):
            p.dma(abs_[u % 2][:, kk:kk + 8, :], actT[k0 + kk:k0 + kk + 8, :, tb:tb + TB].rearrange("k d n -> d k n"))
            p.dma_cast(wts[u % 2][:, kk:kk + 8, 0:mw],
                       w_src[(k0 + kk) * 128:(k0 + kk + 8) * 128, mb:mb + mw].rearrange("(k d) m -> d k m", d=128))

    with p.par():
        loads(0)
    for u, (tb, mb, k0) in enumerate(units):
        ab, wt = abs_[u % 2], wts[u % 2]
        mw = min(512, M - mb)
        with p.par():
            p.flush_deferred()
            if pre is not None:
                npc = (KC + KP - 1) // KP
                kpi = k0 // KP
                for t in range(NT):
                    if (t * npc) // NT == kpi:
                        pre(t, tb + t * 128, 128, mb, mw)
            if u + 1 < len(units):
                loads(u + 1)
            for t in range(NT):
                for kc in range(KP):
                    p.pe("matmul", ps[:, t, 0:mw], ab[:, kc, t * 128:(t + 1) * 128], wt[:, kc, 0:mw],
                         start=(k0 == 0 and kc == 0), stop=(k0 + kc == KC - 1))
        if k0 + KP >= KC:
            for t in range(NT):
                epi(ps[:, t, 0:mw], tb + t * 128, 128, mb, mw, t)
                if t < NT - 1:
                    p.flush_deferred()
    p.flush_deferred()


def stage_peer_ut(p, nc, ctx, U_src, UT_d, NE, D, ident):
    KC = D // 128
    ur = ctx.enter_context(nc.sbuf_tensor("pu_r", [128, D], BF16))
    us = ctx.enter_context(nc.sbuf_tensor("pu_s", [128, KC, 512], BF16))
    pt = ctx.enter_context(nc.psum_tensor("pu_p", [128, 8, 128], BF16))
    for e0 in range(0, NE, 512):
        for eb in range(4):
            p.dma_cast(ur[:], U_src[e0 + eb * 128:e0 + (eb + 1) * 128, :])
            for c0 in range(0, KC, 8):
                nb = min(8, KC - c0)
                with p.par():
                    for c in range(nb):
                        p.pe("transpose", pt[:, c, :], ur[:, (c0 + c) * 128:(c0 + c + 1) * 128], ident[:])
                p.v("tensor_copy", us[:, c0:c0 + nb, eb * 128:(eb + 1) * 128], pt[:, 0:nb, :])
        p.dma(UT_d[:, e0:e0 + 512].rearrange("(k d) e -> d k e", d=128), us[:])


def stage_peer_gate(p, nc, ctx, qT_d, skT_src, G_d, N, PH, tag=""):
    HC = 2 * PH
    mk = lambda n, sh, dt: ctx.enter_context(nc.sbuf_tensor(nc.make_name("pg_" + n + tag, True), sh, dt))
    skT = mk("sk", [128, HC, 128], BF16)
    qt = mk("q", [128, HC, 128], BF16)
    S = mk("S", [128, HC, 128], F32)
    wk = mk("wk", [128, 256], F32)
    top1 = mk("t1", [128, HC, 16], F32)
    cand = mk("cd", [128, PH, 256], F32)
    top2 = mk("t2", [128, PH, 16], F32)
    st = mk("st", [128, 4, PH], F32)
    ex = mk("ex", [128, PH, 16], F32)
    HB = min(4, PH)
    T1 = mk("T1", [128, HB, 16, 128], F32)
    Mk = mk("Mk", [128, HB, 16, 128], F32)
    Gf = mk("Gf", [128, 16, 128], F32)
    Gb = mk("Gb", [128, 128 * 128], BF16)
    ps = ctx.enter_context(nc.psum_tensor(nc.make_name("pg_ps" + tag, True), [128, HC, 128], F32))
    p.dma_cast(skT[:], skT_src)
    for t0 in range(0, N, 128):
        p.dma(qt[:], qT_d[:, :, t0:t0 + 128].rearrange("c q n -> q c n"))
        with p.par():
            for hc in range(HC):
                p.pe("matmul", ps[:, hc, :], qt[:, hc, :], skT[:, hc, :], start=True, stop=True)
        p.v("tensor_copy", S[:], ps[:])
        for hc in range(HC):
            p.v("max", out=top1[:, hc, 0:8], in_=S[:, hc, :])
            p.v("match_replace", out=wk[:, 0:128], in_to_replace=top1[:, hc, 0:8], in_values=S[:, hc, :], imm_value=-1e30)
            p.v("max", out=top1[:, hc, 8:16], in_=wk[:, 0:128])
        p.v("tensor_tensor", apv(cand, 0, [[256, PH], [16, 16], [1, 16]]), apv(top1, 0, [[32, PH], [1, 16], [0, 16]]),
            apv(top1, 16, [[32, PH], [0, 16], [1, 16]]), ALU.add)
        for h in range(PH):
            p.v("max", out=top2[:, h, 0:8], in_=cand[:, h, :])
            p.v("match_replace", out=wk[:], in_to_replace=top2[:, h, 0:8], in_values=cand[:, h, :], imm_value=-1e30)
            p.v("max", out=top2[:, h, 8:16], in_=wk[:])
        p.v("tensor_copy", st[:, 0, :], top2[:, :, 15])
        p.v("tensor_tensor", ex[:], top2[:], apv(top2, 0, [[16, PH], [0, 16]]), ALU.subtract)
        p.a("activation", ex[:], ex[:], ACT.Exp)
        p.v("tensor_reduce", st[:, 2, :], ex[:], AX.X, ALU.add)
        p.a("activation", st[:, 3, :], st[:, 2, :], ACT.Ln)
        p.v("tensor_tensor", st[:, 1, :], st[:, 3, :], top2[:, :, 0], ALU.add)
        p.v("tensor_scalar", st[:, 1, :], st[:, 1, :], -1.0, None, ALU.mult)
        for g0 in range(0, 128, 16):
            for hb in range(0, PH, HB):
                hs = list(range(hb, min(PH, hb + HB)))
                with p.par():
                    for h in hs:
                        p.v("tensor_tensor", Mk[:, h - hb], apv(S, 2 * h * 128 + g0, [[1, 16], [0, 128]]),
                            apv(S, (2 * h + 1) * 128, [[0, 16], [1, 128]]), ALU.add)
                with p.par():
                    for h in hs:
                        p.a("activation", T1[:, h - hb], Mk[:, h - hb], ACT.Exp, bias=st[:, 1, h:h + 1])
                with p.par():
                    for h in hs:
                        if h == 0:
                            p.v("scalar_tensor_tensor", Gf[:], Mk[:, 0], st[:, 0, 0:1], T1[:, 0], ALU.is_ge, ALU.mult)
                        else:
                            p.v("scalar_tensor_tensor", T1[:, h - hb], Mk[:, h - hb], st[:, 0, h:h + 1], T1[:, h - hb],
                                ALU.is_ge, ALU.mult)
                            p.v("tensor_tensor", Gf[:], Gf[:], T1[:, h - hb], ALU.add)
            p.a("copy", Gb[:, g0 * 128:(g0 + 16) * 128], Gf[:].rearrange("p a b -> p (a b)"))
        with p.par():
            for c in range(0, 128 * 128, 4096):
                p.dma(G_d[t0:t0 + 128, c:c + 4096], Gb[:, c:c + 4096])


def stage_norm_tok(p, nc, ctx, x_src, g_src, y_dst, N, D):
    gb = ctx.enter_context(nc.sbuf_tensor(nc.make_name("fgb", True), [128, D], F32))
    xt = ctx.enter_context(nc.sbuf_tensor(nc.make_name("fxt", True), [128, D], F32))
    xn = ctx.enter_context(nc.sbuf_tensor(nc.make_name("fxn", True), [128, D], F32))
    ss = ctx.enter_context(nc.sbuf_tensor(nc.make_name("fss", True), [128, 2], F32))
    p.dma(gb[:], g_src.partition_broadcast(128))
    for t0 in range(0, N, 128):
        p.dma(xt[:], x_src[t0:t0 + 128, :])
        p.a("activation", xn[:], xt[:], ACT.Square, accum_out=ss[:, 0:1])
        p.a("activation", ss[:, 1:2], ss[:, 0:1], ACT.Sqrt, bias=EPS, scale=1.0 / D)
        p.v("reciprocal", ss[:, 1:2], ss[:, 1:2])
        p.v("scalar_tensor_tensor", xn[:], xt[:], ss[:, 1:2], gb[:], ALU.mult, ALU.mult)
        p.dma(y_dst[t0:t0 + 128, :], xn[:])


def build_program(cfg):
    from contextlib import ExitStack
    D = cfg["D"]; NOWN = cfg["NOWN"]; SEGL = cfg["SEGL"]; LH = cfg["LH"]; NKV = LH + SEGL
    NH = cfg["NH"]; NKVH = cfg["NKVH"]; SW = cfg["SW"]; PH = cfg["PH"]; NE = cfg["NE"]
    NP = SW // 32; NJ = SW // 128
    AW = NH * 128; KW = NKVH * 128
    INW = AW + 2 * KW + SW + 2 * D
    cU = AW + 2 * KW; cG = cU + SW
    DC = D // 128
    nc = bass.Bass("TRN2", target_bir_lowering=False)
    din = lambda n, sh: nc.dram_tensor(n, sh, F32, kind="ExternalInput").ap()
    x_own = din("x_own", [NOWN, D]); x_kv = din("x_kv", [NKV, D])
    norm_mix = din("norm_mix", [D]); norm_ffn = din("norm_ffn", [D]); norm_final = din("norm_final", [D])
    w_in = din("w_in", [D, INW]); gq = din("q_norm", [128]); gk = din("k_norm", [128])
    cos_q = din("cos_q", [NOWN, 64]); sin_q = din("sin_q", [NOWN, 64])
    cos_k = din("cos_k", [NKV, 64]); sin_k = din("sin_k", [NKV, 64])
    ident_in = din("ident", [128, 128]); oh_src = din("onehot", [8])
    prm = dict(Ar=din("ssm_Ar", [128, 2, NP]), Ai=din("ssm_Ai", [128, 2, NP]), dt=din("ssm_dt", [128, 2, NP]),
               Bt=din("ssm_Bt", [128, 2, 2, NJ, 128]), Bt3=din("ssm_Bt3", [128, 2, 2, NJ, 128]),
               Ct=din("ssm_Ct", [128, 2, 2, NP, 64]), Dt=din("ssm_Dt", [128, NJ]))
    w_glu = din("w_glu", [SW, SW]); w_up_attn = din("w_up_attn", [AW, D]); w_up_ssm = din("w_up_ssm", [SW, D])
    w_out = din("w_out", [D, D]); w_query = din("peer_w_query", [D, PH * 256])
    skT_src = din("peer_skT", [128, 2 * PH, 128]); U_src = din("peer_u", [NE, D]); V_src = din("peer_v", [NE, D])
    dbg = cfg.get("debug", False)
    y_out = nc.dram_tensor("y_out", [NOWN, D], F32, kind="ExternalOutput").ap()

    def scr(n, sh, dt=BF16):
        return nc.dram_tensor(n, sh, dt, kind=("ExternalOutput" if dbg else "Internal")).ap()
    hT_own = scr("hT_own", [DC, 128, NOWN]); hT_kv = scr("hT_kv", [DC, 128, NKV])
    qT = scr("qT", [NH, 128, NOWN]); kT = scr("kT", [NKVH, 128, NKV]); vt = scr("vt", [NKV, KW])
    attnT = scr("attnT", [NH, 128, NOWN])
    uT_hist = scr("uT_hist", [NJ, 128, LH]); uT_own = scr("uT_own", [NJ, 128, NOWN])
    zT = scr("zT", [NJ, 128, NOWN]); y_scr = scr("y_scr", [NJ, 128, NOWN], F32)
    soT = scr("soT", [NJ, 128, NOWN]); sgT = scr("sgT", [2 * DC, 128, NOWN])
    mF = scr("mF", [DC, 128, NOWN], F32); mergedT = scr("mergedT", [DC, 128, NOWN])
    x2 = scr("x2", [NOWN, D], F32); tT = scr("tT", [DC, 128, NOWN]); qpT = scr("qpT", [2 * PH, 128, NOWN])
    G_d = scr("G_d", [NOWN, NE]); UT_d = scr("UT_d", [D, NE]); AT_d = scr("AT_d", [NE // 128, 128, NOWN])
    x3 = scr("x3", [NOWN, D], F32)
    with ExitStack() as ctx:
        G = ctx.enter_context(nc.semaphore("G"))
        ident = ctx.enter_context(nc.sbuf_tensor("identb", [128, 128], BF16))
        p = Prog(nc)
        p.dma_cast(ident[:], ident_in)

        def store_T_epi(sctx, dst, dt, name, func=None):
            stt = sctx.enter_context(nc.sbuf_tensor(name, [128, 512], dt))

            def epi(ps, tok0, ntok, m0, nm):
                if func is None:
                    p.v("tensor_copy", stt[:, 0:ntok], ps)
                else:
                    p.a("activation", stt[:, 0:ntok], ps, func)
                p.dma_deferred(dst[m0 // 128, :, tok0:tok0 + ntok], stt[:, 0:ntok])
            return epi

        with ExitStack() as sctx:
            stage_norm_T(p, nc, sctx, x_own, norm_mix, hT_own, NOWN, D, ident)
        with ExitStack() as sctx:
            stage_norm_T(p, nc, sctx, x_kv, norm_mix, hT_kv, NKV, D, ident)
        with ExitStack() as sctx:
            epq = QKEpi(p, nc, sctx, gq, cos_q, sin_q, qT, ident, "q")
            stage_gemm(p, nc, sctx, hT_own, w_in[:, 0:AW], NOWN, D, AW, "T", epq, "q")
        with ExitStack() as sctx:
            epk = QKEpi(p, nc, sctx, gk, cos_k, sin_k, kT, ident, "k")
            stage_gemm(p, nc, sctx, hT_kv, w_in[:, AW:AW + KW], NKV, D, KW, "T", epk, "k")
        with ExitStack() as sctx:
            st = sctx.enter_context(nc.sbuf_tensor("stv", [128, 512], BF16))

            def epv(ps, tok0, ntok, m0, nm):
                p.v("tensor_copy", st[:, 0:nm], ps)
                p.dma_deferred(vt[tok0:tok0 + ntok, m0:m0 + nm], st[:, 0:nm])
            stage_gemm(p, nc, sctx, hT_kv, w_in[:, AW + KW:AW + 2 * KW], NKV, D, KW, "T", epv, "v")
        with ExitStack() as sctx:
            stage_attention(p, nc, sctx, qT, kT, vt, attnT, gq, gk, cfg["segs"], NH, NKVH, "a")
        if LH > SEGL:
            with ExitStack() as sctx:
                stage_gemm(p, nc, sctx, hT_kv[:, :, 0:LH], w_in[:, cU:cU + SW], LH, D, SW, "F",
                           store_T_epi(sctx, uT_hist, BF16, "st_uh"), "uh")
        with ExitStack() as sctx:
            stage_gemm(p, nc, sctx, hT_own, w_in[:, cU:cU + SW], NOWN, D, SW, "F",
                       store_T_epi(sctx, uT_own, BF16, "st_uo"), "uo")
        with ExitStack() as sctx:
            stage_ssm(p, nc, sctx, uT_hist, uT_own, zT, y_scr, prm, oh_src, LH, SEGL,
                      [(0, True), (SEGL, False)], NP, NJ)
        with ExitStack() as sctx:
            sg = sctx.enter_context(nc.sbuf_tensor("glu_s", [128, 512], F32))
            zt_ = sctx.enter_context(nc.sbuf_tensor("glu_z", [128, 512], BF16))
            so_ = sctx.enter_context(nc.sbuf_tensor("glu_o", [128, 512], BF16))

            def pre_glu(tok0, ntok, m0, nm):
                p.dma(zt_[:, 0:ntok], zT[m0 // 128, :, tok0:tok0 + ntok])

            def epi_glu(ps, tok0, ntok, m0, nm):
                p.a("activation", sg[:, 0:ntok], ps, ACT.Sigmoid)
                p.v("tensor_tensor", so_[:, 0:ntok], zt_[:, 0:ntok], sg[:, 0:ntok], ALU.mult)
                p.dma_deferred(soT[m0 // 128, :, tok0:tok0 + ntok], so_[:, 0:ntok])
            stage_gemm(p, nc, sctx, zT, w_glu, NOWN, SW, SW, "F", epi_glu, "glu", pre=pre_glu)
        with ExitStack() as sctx:
            stage_gemm(p, nc, sctx, hT_own, w_in[:, cG:cG + 2 * D], NOWN, D, 2 * D, "F",
                       store_T_epi(sctx, sgT, BF16, "st_sg", ACT.Sigmoid), "sg")
        with ExitStack() as sctx:
            ga = sctx.enter_context(nc.sbuf_tensor("ua_g", [128, 512], BF16))
            mo = sctx.enter_context(nc.sbuf_tensor("ua_m", [128, 512], F32))

            def pre_ua(tok0, ntok, m0, nm):
                p.dma(ga[:, 0:ntok], sgT[m0 // 128, :, tok0:tok0 + ntok])

            def epi_ua(ps, tok0, ntok, m0, nm):
                p.v("tensor_tensor", mo[:, 0:ntok], ps, ga[:, 0:ntok], ALU.mult)
                p.dma_deferred(mF[m0 // 128, :, tok0:tok0 + ntok], mo[:, 0:ntok])
            stage_gemm(p, nc, sctx, attnT, w_up_attn, NOWN, AW, D, "F", epi_ua, "ua", pre=pre_ua)
        with ExitStack() as sctx:
            gs = sctx.enter_context(nc.sbuf_tensor("us_g", [128, 512], BF16))
            mp = sctx.enter_context(nc.sbuf_tensor("us_p", [128, 512], F32))
            mo2 = sctx.enter_context(nc.sbuf_tensor("us_m", [128, 512], F32))
            mb_ = sctx.enter_context(nc.sbuf_tensor("us_b", [128, 512], BF16))

            def pre_us(tok0, ntok, m0, nm):
                p.dma(gs[:, 0:ntok], sgT[DC + m0 // 128, :, tok0:tok0 + ntok])
                p.dma(mp[:, 0:ntok], mF[m0 // 128, :, tok0:tok0 + ntok])

            def epi_us(ps, tok0, ntok, m0, nm):
                p.v("tensor_tensor", mo2[:, 0:ntok], ps, gs[:, 0:ntok], ALU.mult)
                p.v("tensor_tensor", mb_[:, 0:ntok], mo2[:, 0:ntok], mp[:, 0:ntok], ALU.add)
                p.dma_deferred(mergedT[m0 // 128, :, tok0:tok0 + ntok], mb_[:, 0:ntok])
            stage_gemm(p, nc, sctx, soT, w_up_ssm, NOWN, SW, D, "F", epi_us, "us", pre=pre_us)
        with ExitStack() as sctx:
            xr = sctx.enter_context(nc.sbuf_tensor("wo_x", [128, 512], F32))
            xo = sctx.enter_context(nc.sbuf_tensor("wo_o", [128, 512], F32))

            def pre_wo(tok0, ntok, m0, nm):
                p.dma(xr[:, 0:nm], x_own[tok0:tok0 + ntok, m0:m0 + nm])

            def epi_wo(ps, tok0, ntok, m0, nm):
                p.v("tensor_tensor", xo[:, 0:nm], ps, xr[:, 0:nm], ALU.add)
                p.dma_deferred(x2[tok0:tok0 + ntok, m0:m0 + nm], xo[:, 0:nm])
            stage_gemm(p, nc, sctx, mergedT, w_out, NOWN, D, D, "T", epi_wo, "wo", pre=pre_wo)
        with ExitStack() as sctx:
            stage_norm_T(p, nc, sctx, x2, norm_ffn, tT, NOWN, D, ident)
        with ExitStack() as sctx:
            stage_peer_ut(p, nc, sctx, U_src, UT_d, NE, D, ident)
        with ExitStack() as sctx:
            stage_gemm(p, nc, sctx, tT, w_query, NOWN, D, PH * 256, "F",
                       store_T_epi(sctx, qpT, BF16, "st_qp"), "qp")
        with ExitStack() as sctx:
            stage_peer_gate(p, nc, sctx, qpT, skT_src, G_d, NOWN, PH)
        with ExitStack() as sctx:
            ge = sctx.enter_context(nc.sbuf_tensor("pa_ge", [128, 512], F32))
            gl = sctx.enter_context(nc.sbuf_tensor("pa_gl", [128, 512], BF16))
            ab_ = sctx.enter_context(nc.sbuf_tensor("pa_a", [128, 512], BF16))
            at_ = sctx.enter_context(nc.sbuf_tensor("pa_at", [128, 4, 128], BF16))
            pta = sctx.enter_context(nc.psum_tensor("pa_pt", [128, 4, 128], BF16))

            def pre_pa(tok0, ntok, m0, nm):
                p.dma(gl[:, 0:nm], G_d[tok0:tok0 + ntok, m0:m0 + nm])

            def epi_pa(ps, tok0, ntok, m0, nm):
                p.a("activation", ge[:, 0:nm], ps, ACT.Gelu_apprx_tanh)
                p.v("tensor_tensor", ab_[:, 0:nm], ge[:, 0:nm], gl[:, 0:nm], ALU.mult)
                nb = nm // 128
                with p.par():
                    for c in range(nb):
                        p.pe("transpose", pta[:, c, :], ab_[:, c * 128:(c + 1) * 128], ident[:])
                p.v("tensor_copy", at_[:, 0:nb, :], pta[:, 0:nb, :])
                p.dma_deferred(AT_d[m0 // 128:m0 // 128 + nb, :, tok0:tok0 + ntok].rearrange("c e t -> e c t"), at_[:, 0:nb, :])
            stage_gemm(p, nc, sctx, tT, UT_d, NOWN, D, NE, "T", epi_pa, "pa", w_bf16=True, pre=pre_pa)
        with ExitStack() as sctx:
            xr2 = sctx.enter_context(nc.sbuf_tensor("pb_x", [128, 8, 512], F32))
            xo2 = sctx.enter_context(nc.sbuf_tensor("pb_o", [128, 512], F32))

            def pre_pb(t, tok0, ntok, m0, nm):
                p.dma(xr2[:, t, 0:nm], x2[tok0:tok0 + ntok, m0:m0 + nm])

            def epi_pb(ps, tok0, ntok, m0, nm, t):
                p.v("tensor_tensor", xo2[:, 0:nm], ps, xr2[:, t, 0:nm], ALU.add)
                p.dma_deferred(x3[tok0:tok0 + ntok, m0:m0 + nm], xo2[:, 0:nm])
            stage_gemm_bigk(p, nc, sctx, AT_d, V_src, NOWN, NE, D, epi_pb, "pb", pre=pre_pb)
        with ExitStack() as sctx:
            stage_norm_tok(p, nc, sctx, x3, norm_final, y_out, NOWN, D)
        p.finish(G)
    return nc


FULL = dict(D=4096, NOWN=4096, SEGL=2048, LH=16384, NH=16, NKVH=4, SW=2048, PH=8, NE=16384,
            segs=[(0, 2048, 0, 16384), (2048, 2048, 16384, 2048)])


def _rope_tables(length):
    rows = length // 64
    row = np.repeat(np.arange(rows), 64).astype(np.float32)
    col = np.tile(np.arange(64), rows).astype(np.float32)
    inv = (10000.0 ** (-np.arange(0, 64, 2, dtype=np.float32) / 64)).astype(np.float32)
    ang = np.concatenate([row[:, None] * inv, col[:, None] * inv], axis=-1).astype(np.float32)
    return np.cos(ang).astype(np.float32), np.sin(ang).astype(np.float32)


def make_in_maps(inputs, cfg, n_cores):
    g = lambda k: np.asarray(inputs[k])
    SEGL, LH = cfg["SEGL"], cfg["LH"]
    xp = g("x_prompt")[0]
    xs = g("x_sample")
    cp, sp = _rope_tables(LH)
    cs, ss = _rope_tables(SEGL)
    cos_k = np.concatenate([cp, cs], 0); sin_k = np.concatenate([sp, ss], 0)
    shared = {
        "norm_mix": np.ascontiguousarray(g("norm_mix")[0]), "norm_ffn": np.ascontiguousarray(g("norm_ffn")[0]),
        "norm_final": np.ascontiguousarray(g("norm_final")),
        "w_in": np.ascontiguousarray(g("w_in")[0]), "q_norm": np.ascontiguousarray(g("q_norm")[0]),
        "k_norm": np.ascontiguousarray(g("k_norm")[0]), "cos_k": cos_k, "sin_k": sin_k,
        "ident": np.eye(128, dtype=np.float32),
        "w_glu": np.ascontiguousarray(g("w_glu")[0]), "w_up_attn": np.ascontiguousarray(g("w_up_attn")[0]),
        "w_up_ssm": np.ascontiguousarray(g("w_up_ssm")[0]), "w_out": np.ascontiguousarray(g("w_out")[0]),
        "peer_w_query": np.ascontiguousarray(g("peer_w_query")[0]),
        "peer_skT": np.ascontiguousarray(g("peer_sub_keys")[0].reshape(-1, 128, 128).transpose(2, 0, 1)),
        "peer_u": np.ascontiguousarray(g("peer_u")[0]), "peer_v": np.ascontiguousarray(g("peer_v")[0]),
    }
    shared.update(ssm_host_layout(g("ssm_A_re")[0], g("ssm_A_im")[0], g("ssm_log_dt")[0], g("ssm_B_re")[0],
                                  g("ssm_B_im")[0], g("ssm_C_re")[0], g("ssm_C_im")[0], g("ssm_D")[0]))
    in_maps = []
    for i in range(n_cores):
        sl = slice(SEGL * i, SEGL * (i + 1))
        m = dict(shared)
        m["x_own"] = np.ascontiguousarray(np.concatenate([xp[sl], xs[i]], axis=0))
        m["x_kv"] = np.ascontiguousarray(np.concatenate([xp, xs[i]], axis=0))
        m["cos_q"] = np.ascontiguousarray(np.concatenate([cp[sl], cs], 0))
        m["sin_q"] = np.ascontiguousarray(np.concatenate([sp[sl], ss], 0))
        oh = np.zeros(8, np.float32); oh[i] = 1.0
        m["onehot"] = oh
        in_maps.append(m)
    return in_maps


def kernel(**inputs):
    cfg = FULL
    nc = build_program(cfg)
    in_maps = make_in_maps(inputs, cfg, 8)
    res = run_bass_kernel_spmd(nc, in_maps, core_ids=list(range(8)))
    S = cfg["SEGL"]
    yp = np.concatenate([r["y_out"][:S] for r in res.results], axis=0)[None]
    ys = np.stack([r["y_out"][S:] for r in res.results], axis=0)
    return (yp.astype(np.float32), ys.astype(np.float32))
```
